# Optimizing a Trainium2 kernel written in Bass

```python
import math
import jax, jax.numpy as jnp
from jax import lax
import numpy as np

D_MODEL = 1024
BATCH = 2
SEQ = 8192
DEPTH = 1

EPS = 1e-6
A_GROUPS = 8
A_GROUP_DIM = D_MODEL // A_GROUPS
A_WIDTH = A_GROUPS * A_GROUP_DIM
CHUNK = 128
B_HEADS = 8
B_HEAD_DIM = D_MODEL // 16
B_V_DIM = 2 * B_HEAD_DIM
QK_WIDTH = B_HEADS * 2 * B_HEAD_DIM
B_WIDTH = B_HEADS * B_V_DIM
Q_BLOCK = 128
IN_COLS = 2 * A_WIDTH + 2 * QK_WIDTH + B_WIDTH
N_GROUPS = 4
EXPERTS_PER_GROUP = 8
N_EXPERTS = N_GROUPS * EXPERTS_PER_GROUP
TOP_K = 2
D_EXPERT = D_MODEL // 4
MOE_BLOCK = 128

kernel_name = "hybrid_gmlp_diffattn_hiermoe"


def rmsnorm(x, g):
    xf = x.astype(jnp.float32)
    r = lax.rsqrt(jnp.mean(xf * xf, axis=-1, keepdims=True) + EPS)
    return (xf * r).astype(x.dtype) * g


def chunked_spatial_gating(u, v, g_v, w_s, b_s):
    bsz, s, _ = u.shape
    nc = s // CHUNK
    v = rmsnorm(v.reshape(bsz, s, A_GROUPS, A_GROUP_DIM), g_v)
    v = v.reshape(bsz, nc, CHUNK, A_GROUPS, A_GROUP_DIM)
    causal = jnp.tril(jnp.ones((CHUNK, CHUNK), dtype=bool))
    ws = jnp.where(causal[None], w_s, 0.0)
    z = jnp.einsum('gts,bcsgd->bctgd', ws, v) + b_s.T[None, None, :, :, None]
    return u * z.reshape(bsz, s, A_WIDTH)


def diff_attention(q, k, v, lam, slopes):
    bsz, nh, _, s, hd = q.shape
    nb = s // Q_BLOCK
    key_pos = jnp.arange(s)
    scale = hd ** -0.5

    def block(i):
        start = i * Q_BLOCK
        qb = lax.dynamic_slice_in_dim(q, start, Q_BLOCK, axis=3)
        sc = jnp.einsum('bhcqd,bhckd->bhcqk', qb, k,
                        preferred_element_type=jnp.float32) * scale
        q_pos = start + jnp.arange(Q_BLOCK)
        dist = (q_pos[:, None] - key_pos[None, :]).astype(jnp.float32)
        alibi = -slopes[:, None, None] * jnp.abs(dist)[None]
        sc = jnp.where((dist >= 0)[None, None, None], sc + alibi[None, :, None], -jnp.inf)
        p = jax.nn.softmax(sc, axis=-1)
        a = p[:, :, 0] - lam * p[:, :, 1]
        return jnp.einsum('bhqk,bhkd->bhqd', a.astype(v.dtype), v)

    out = lax.map(block, jnp.arange(nb))
    return out.transpose(1, 2, 0, 3, 4).reshape(bsz, nh, s, v.shape[-1])


def hierarchical_moe(h, w_rg, b_rg, w_re, b_re, w1, w3, w2):
    bsz, s, d = h.shape
    t = h.reshape(-1, d)
    n_tok = t.shape[0]
    g_prob = jax.nn.softmax((t @ w_rg + b_rg).astype(jnp.float32), axis=-1)
    g_gate, g_idx = lax.top_k(g_prob, 1)
    e_logits = (t @ w_re + b_re).astype(jnp.float32).reshape(n_tok, N_GROUPS, EXPERTS_PER_GROUP)
    e_logits = jnp.take_along_axis(e_logits, g_idx[:, :, None], axis=1)[:, 0]
    e_w, e_idx = lax.top_k(jax.nn.softmax(e_logits, axis=-1), TOP_K)
    e_w = e_w / jnp.sum(e_w, axis=-1, keepdims=True)
    weight = g_gate * e_w
    expert_id = g_idx * EXPERTS_PER_GROUP + e_idx
    combine = jnp.sum(jax.nn.one_hot(expert_id, N_EXPERTS, dtype=jnp.float32)
                      * weight[..., None], axis=1)

    def expert_block(args):
        tk, c = args
        hid = jax.nn.silu(jnp.einsum('td,edf->tef', tk, w1)) * jnp.einsum('td,edf->tef', tk, w3)
        hid = hid * c[:, :, None].astype(hid.dtype)
        return jnp.einsum('tef,efd->td', hid, w2)

    y = lax.map(expert_block, (t.reshape(-1, MOE_BLOCK, d),
                               combine.reshape(-1, MOE_BLOCK, N_EXPERTS)))
    return y.reshape(bsz, s, d)


def setup_inputs(seed: int = 0) -> dict:
    key = jax.random.key(seed)
    ks = jax.random.split(key, 32)
    f32 = jnp.float32
    L, D = DEPTH, D_MODEL

    def nrm(k, shape, scale):
        return jax.random.normal(k, shape, f32) * scale

    def gain(k, shape):
        return 1.0 + 0.02 * jax.random.normal(k, shape, f32)

    return {
        "x": jax.random.normal(ks[0], (BATCH, SEQ, D), f32),
        "norm1_g": gain(ks[1], (L, D)),
        "w_in": nrm(ks[2], (L, D, IN_COLS), D ** -0.5),
        "v_norm_g": gain(ks[3], (L, A_GROUPS, A_GROUP_DIM)),
        "w_s": nrm(ks[4], (L, A_GROUPS, CHUNK, CHUNK), CHUNK ** -0.5),
        "b_s": 1.0 + 0.02 * jax.random.normal(ks[5], (L, A_GROUPS, CHUNK), f32),
        "q_norm_g": gain(ks[6], (L, B_HEAD_DIM)),
        "k_norm_g": gain(ks[7], (L, B_HEAD_DIM)),
        "lambda_q1": nrm(ks[8], (L, B_HEAD_DIM), 0.1),
        "lambda_k1": nrm(ks[9], (L, B_HEAD_DIM), 0.1),
        "lambda_q2": nrm(ks[10], (L, B_HEAD_DIM), 0.1),
        "lambda_k2": nrm(ks[11], (L, B_HEAD_DIM), 0.1),
        "sub_norm_g": gain(ks[12], (L, B_V_DIM)),
        "w_up_a": nrm(ks[13], (L, A_WIDTH, D), A_WIDTH ** -0.5),
        "w_up_b": nrm(ks[14], (L, B_WIDTH, D), B_WIDTH ** -0.5),
        "w_gate": nrm(ks[15], (L, D, 2 * D), D ** -0.5),
        "b_gate": nrm(ks[16], (L, 2 * D), 0.02),
        "w_out": nrm(ks[17], (L, D, D), D ** -0.5),
        "norm2_g": gain(ks[18], (L, D)),
        "w_rg": nrm(ks[19], (L, D, N_GROUPS), D ** -0.5),
        "b_rg": nrm(ks[20], (L, N_GROUPS), 0.01),
        "w_re": nrm(ks[21], (L, D, N_EXPERTS), D ** -0.5),
        "b_re": nrm(ks[22], (L, N_EXPERTS), 0.01),
        "w1": nrm(ks[23], (L, N_EXPERTS, D, D_EXPERT), D ** -0.5),
        "w3": nrm(ks[24], (L, N_EXPERTS, D, D_EXPERT), D ** -0.5),
        "w2": nrm(ks[25], (L, N_EXPERTS, D_EXPERT, D), D_EXPERT ** -0.5),
    }


def reference(x, norm1_g, w_in, v_norm_g, w_s, b_s, q_norm_g, k_norm_g,
              lambda_q1, lambda_k1, lambda_q2, lambda_k2, sub_norm_g,
              w_up_a, w_up_b, w_gate, b_gate, w_out, norm2_g,
              w_rg, b_rg, w_re, b_re, w1, w3, w2):
    bsz, s, d = x.shape
    slopes = jnp.exp2(-8.0 * (jnp.arange(B_HEADS) + 1) / B_HEADS)
    splits = [A_WIDTH, 2 * A_WIDTH, 2 * A_WIDTH + QK_WIDTH, 2 * A_WIDTH + 2 * QK_WIDTH]
    for l in range(DEPTH):
        lam_init = 0.8 - 0.6 * math.exp(-0.3 * l)
        h = rmsnorm(x, norm1_g[l])
        proj = h @ w_in[l]
        u, v_a, q, k, v_b = jnp.split(proj, splits, axis=-1)

        y_a = chunked_spatial_gating(u, v_a, v_norm_g[l], w_s[l], b_s[l])

        q = rmsnorm(q.reshape(bsz, s, B_HEADS, 2, B_HEAD_DIM), q_norm_g[l]).transpose(0, 2, 3, 1, 4)
        k = rmsnorm(k.reshape(bsz, s, B_HEADS, 2, B_HEAD_DIM), k_norm_g[l]).transpose(0, 2, 3, 1, 4)
        vb = v_b.reshape(bsz, s, B_HEADS, B_V_DIM).transpose(0, 2, 1, 3)
        lam = (jnp.exp(jnp.sum(lambda_q1[l].astype(jnp.float32) * lambda_k1[l].astype(jnp.float32)))
               - jnp.exp(jnp.sum(lambda_q2[l].astype(jnp.float32) * lambda_k2[l].astype(jnp.float32)))
               + lam_init)
        o = diff_attention(q, k, vb, lam, slopes)
        o = rmsnorm(o, sub_norm_g[l]) * (1.0 - lam_init)
        y_b = o.transpose(0, 2, 1, 3).reshape(bsz, s, B_WIDTH)

        gates = jax.nn.sigmoid(h @ w_gate[l] + b_gate[l])
        g_a, g_b = jnp.split(gates, 2, axis=-1)
        merged = g_a * (y_a @ w_up_a[l]) + g_b * (y_b @ w_up_b[l])
        x = x + merged @ w_out[l]

        h2 = rmsnorm(x, norm2_g[l])
        x = x + hierarchical_moe(h2, w_rg[l], b_rg[l], w_re[l], b_re[l], w1[l], w3[l], w2[l])
    return x
```

```python
import os
import numpy as np
import concourse.bass as bass
import concourse.mybir as mybir
from concourse.bass_utils import run_bass_kernel_spmd

F32 = mybir.dt.float32
BF16 = mybir.dt.bfloat16
AF = mybir.ActivationFunctionType
ALU = mybir.AluOpType
AX = mybir.AxisListType

D = 1024
SEQ = 8192
NSB = 16
KMAX = [4, 8, 12, 16]
EPS = 1e-6
NE = 32
WH = [256, 512, 512, 512, 512, 512, 512, 512]
SLOPES = [2.0 ** (-(h + 1)) for h in range(8)]


def abias_layout():
    off = {}
    n = 0
    for h in range(8):
        nsub = 512 // WH[h]
        for s in range(4):
            for r in range(nsub):
                off[(h, s, r)] = n
                n += 4 * KMAX[s]
    return off, n


ABOFF, NAB = abias_layout()


class Buf:
    def __init__(self, name, dram=False):
        self.name = name
        self.dram = dram
        self.w = {}
        self.r = {}
        self.dsem = None
        self.dcnt = 0


class Sched:
    CE = ("pe", "act", "dve", "pool")

    def __init__(self, nc):
        self.nc = nc
        self.E = {"pe": nc.tensor, "act": nc.scalar, "dve": nc.vector, "pool": nc.gpsimd, "sp": nc.sync}
        self.sem = {k: nc.alloc_semaphore(name="cs_" + k) for k in self.CE}
        self.n = {k: 0 for k in self.CE}
        self.last = {k: None for k in self.CE}
        self.sig = {k: [] for k in self.CE}
        self.seen = {k: {} for k in self.E}
        self.dsems = []

    def _value_for(self, F, i):
        sl = self.sig[F]
        lo, hi = 0, len(sl)
        while lo < hi:
            mid = (lo + hi) // 2
            if sl[mid] >= i:
                hi = mid
            else:
                lo = mid + 1
        if lo < len(sl):
            return lo + 1
        self._signal_last(F)
        return len(sl)

    def _signal_last(self, F):
        if self.sig[F] and self.sig[F][-1] == self.n[F]:
            return
        self.last[F].then_inc(self.sem[F], 1)
        self.sig[F].append(self.n[F])

    def _wait(self, E, tok):
        if tok is None:
            return
        if tok[0] == "dma":
            _, sem, val, key = tok
            if self.seen[E].get(key, 0) >= val:
                return
            self.E[E].wait_ge(sem, val)
            self.seen[E][key] = val
            return
        F, i = tok
        if F == E and E == "pe":
            return
        val = self._value_for(F, i)
        if self.seen[E].get(F, 0) >= val:
            return
        self.E[E].wait_ge(self.sem[F], val)
        self.seen[E][F] = val

    def _deps(self, E, R, W, join=False):
        for b in R:
            for t in list(b.w.values()):
                self._wait(E, t)
        for b in W:
            if not join:
                for t in list(b.w.values()):
                    self._wait(E, t)
            for t in list(b.r.values()):
                self._wait(E, t)

    def op(self, E, fn, R=(), W=(), sig=False):
        self._deps(E, R, W)
        ins = fn()
        self.n[E] += 1
        self.last[E] = ins
        tok = (E, self.n[E])
        for b in R:
            b.r[E] = tok
        for b in W:
            b.w = {E: tok}
            b.r = {}
        if sig:
            self._signal_last(E)
        return ins

    def dma(self, q, out, in_, R=(), W=(), join=False):
        self._deps(q, R, W, join=join)
        sb = W[0] if W else R[0]
        if W and W[0].dram and R:
            sb = R[0]
        if q == "pool":
            self.nsw = getattr(self, "nsw", 0) + 1
            sb = Buf("sw%d_%s" % (self.nsw, sb.name))
            assert not join
        if sb.dsem is None:
            sb.dsem = self.nc.alloc_semaphore(name="ds_" + sb.name)
            self.dsems.append(sb)
        sb.dcnt += 16
        self.E[q].dma_start(out=out, in_=in_).then_inc(sb.dsem, 16)
        tok = ("dma", sb.dsem, sb.dcnt, sb.name)
        for b in R:
            b.r[("dma", sb.name)] = tok
        for b in W:
            if join:
                b.w[("dma", sb.name)] = tok
            else:
                b.w = {("dma", sb.name): tok}
                b.r = {}
        return tok

    def barrier(self):
        for F in self.CE:
            if self.last[F] is not None:
                self._signal_last(F)
        for E in self.E:
            for F in self.CE:
                if self.last[F] is None or F == E:
                    continue
                val = len(self.sig[F])
                if self.seen[E].get(F, 0) < val:
                    self.E[E].wait_ge(self.sem[F], val)
                    self.seen[E][F] = val
            for sb in self.dsems:
                if self.seen[E].get(sb.name, 0) < sb.dcnt:
                    self.E[E].wait_ge(sb.dsem, sb.dcnt)
                    self.seen[E][sb.name] = sb.dcnt

    def finish(self, toks):
        for t in toks:
            self._wait("sp", t)


def build_nc(upto=None, debug=False):
    nc = bass.Bass("TRN2", target_bir_lowering=False)
    S = Sched(nc)

    def din(name, shape, dt=F32):
        return nc.dram_tensor(name, list(shape), dt, kind="ExternalInput").ap()

    xf = din("xf", [SEQ, D])
    xo = din("xo", [2048, D])
    w_in = din("w_in", [D, 5120])
    w_gate = din("w_gate", [D, 2048])
    w_up_a = din("w_up_a", [D, D])
    w_up_b = din("w_up_b", [D, D])
    w_out = din("w_out", [D, D])
    ewp = din("ewp", [NE, 128, 6144])
    c_ident = din("c_ident", [128, 128])
    c_g1 = din("c_g1", [128, 8])
    c_g2 = din("c_g2", [128, 8])
    c_gqk = din("c_gqk", [128, 2])
    c_gv = din("c_gv", [128, 1024])
    c_wsT = din("c_wsT", [128, 8, 128])
    c_triu = din("c_triu", [128, 128])
    c_bs = din("c_bs", [128, 8, 128])
    c_bgate = din("c_bgate", [128, 16])
    c_gsub = din("c_gsub", [128, 1])
    c_lam = din("c_lam", [128, 4, 64])
    c_br = din("c_br", [128, 36])
    c_wr = din("c_wr", [128, 8, 36])
    c_sel = din("c_sel", [64, NE, 128])
    c_tri4 = din("c_tri4", [128, 4, 512])
    c_mcoef = din("c_mcoef", [128, 32])
    c_abias = din("c_abias", [128, NAB])
    out = nc.dram_tensor("out", [2048, D], F32, kind="ExternalOutput").ap()

    def dscr(name, shape, dt=BF16):
        return nc.dram_tensor(name, list(shape), dt, kind=("ExternalOutput" if debug else "Internal")).ap()

    hTs = dscr("hTs", [4, 128, 8 * 512])
    QTs = dscr("QTs", [4, 128, 8 * 512])
    yaTs = dscr("yaTs", [4, 128, 8 * 512])
    ATs = dscr("ATs", [4, 128, 8 * 512])
    mTs = dscr("mTs", [4, 128, 8 * 512])
    KTs = dscr("KTs", [NSB, 128, 8 * 512])
    Vs = dscr("Vs", [NSB, 128, 8 * 4 * 129])

    def sb(name, shape, dt, off):
        return nc.alloc_sbuf_tensor_at(name, list(shape), dt, offset=off)

    BASE = 16512
    R_A = BASE
    R_B = R_A + 65536
    R_W = R_B + 32768
    R_C = R_W + 49152
    R_S = R_C + 32768

    co = [R_C]

    def calloc(name, shape, dt):
        nbytes = int(np.prod(shape[1:])) * (4 if dt == F32 else 2)
        nbytes = (nbytes + 31) // 32 * 32
        t = sb(name, shape, dt, co[0])
        co[0] += nbytes
        assert co[0] <= R_S, name
        return t

    ident_f = calloc("ident_f", [128, 128], F32)
    ident_b = calloc("ident_b", [128, 128], BF16)
    g1c = calloc("g1c", [128, 8], F32)
    g2c = calloc("g2c", [128, 8], F32)
    gqk = calloc("gqk", [128, 2], F32)
    gv_bc = calloc("gv_bc", [128, 1024], F32)
    wsT_m = calloc("wsT_m", [128, 8, 128], BF16)
    bs_t = calloc("bs_t", [128, 8, 128], F32)
    bgate = calloc("bgate", [128, 16], F32)
    gsub8 = calloc("gsub8", [128, 1], F32)
    ones_b = calloc("ones_b", [128, 128], BF16)
    br_t = calloc("br_t", [128, 36], F32)
    wr_t = calloc("wr_t", [128, 8, 36], F32)
    sel_t = calloc("sel_t", [64, NE, 128], BF16)
    mcoef = calloc("mcoef", [128, 32], F32)
    zeros_b = calloc("zeros_b", [128, 640], BF16)
    epsT = calloc("epsT", [128, 1], F32)
    neglam = calloc("neglam", [128, 1], F32)
    cT = calloc("cT", [64, 2048], BF16)
    CONST = Buf("const")

    PA = nc.alloc_psum_tensor("PA", [128, 1024], F32)
    PB = nc.alloc_psum_tensor("PB", [128, 1024], F32)
    PC = nc.alloc_psum_tensor("PC", [128, 1024], F32)
    PD = nc.alloc_psum_tensor("PD", [128, 1024], F32)
    bPA0, bPA1, bPB0, bPB1, bPC0, bPC1, bPD, bPD1 = [Buf("ps%d" % i) for i in range(8)]

    def MM(out_, lhsT, rhs, start, stop, R, W, sig=False, **kw):
        return S.op("pe", lambda: nc.tensor.matmul(out_, lhsT=lhsT, rhs=rhs, start=start, stop=stop, **kw), R, W, sig)

    def TR(out_, in_, ident, R, W, sig=False):
        return S.op("pe", lambda: nc.tensor.transpose(out_, in_, ident), R, W, sig)

    def ACT(out_, in_, func, R, W, bias=None, scale=None, sig=False):
        kw = {}
        if bias is not None:
            kw["bias"] = bias
        if scale is not None:
            kw["scale"] = scale
        return S.op("act", lambda: nc.scalar.activation(out=out_, in_=in_, func=func, **kw), R, W, sig)

    def V(name, R, W, sig=False, eng="dve", **kw):
        e = nc.vector if eng == "dve" else nc.gpsimd
        return S.op(eng, lambda: getattr(e, name)(**kw), R, W, sig)

    lam_t = sb("lam_t", [128, 4, 64], F32, R_A)
    lam_p = sb("lam_p", [128, 2, 64], F32, R_A + 1024)
    lam_s = sb("lam_s", [128, 2], F32, R_A + 1536)
    lam_e = sb("lam_e", [128, 2], F32, R_A + 1568)
    wsT_f = sb("wsT_f", [128, 8, 128], F32, R_A + 2048)
    triu_f = sb("triu_f", [128, 128], F32, R_A + 6144)
    gsub_f = sb("gsub_f", [128, 1], F32, R_A + 6656)
    SETUP = Buf("setup")
    for dst, src in [(ident_f, c_ident), (g1c, c_g1), (g2c, c_g2), (gqk, c_gqk), (gv_bc, c_gv), (bs_t, c_bs),
                     (bgate, c_bgate), (br_t, c_br), (wr_t, c_wr), (mcoef, c_mcoef)]:
        S.dma("sp", dst[:], src, W=[CONST], join=True)
    for dst, src in [(lam_t, c_lam), (wsT_f, c_wsT), (triu_f, c_triu), (gsub_f, c_gsub)]:
        S.dma("sp", dst[:], src, W=[SETUP], join=True)
    CSEL = Buf("csel")
    CIDB = Buf("cidb")
    S.dma("pool", sel_t[:], c_sel, W=[CSEL])
    S.dma("pool", ident_b[:], c_ident, W=[CIDB])
    C2 = Buf("const2")
    V("memset", [], [C2], ap=zeros_b[:], constant=0.0)
    V("memset", [], [C2], ap=epsT[:], constant=EPS)
    V("memset", [], [C2], ap=ones_b[:], constant=1.0)
    V("tensor_tensor", [SETUP], [C2], out=wsT_m[:], in0=wsT_f[:], in1=triu_f[:].unsqueeze(1).to_broadcast([128, 8, 128]), op=ALU.mult)
    V("tensor_scalar", [SETUP], [C2], out=gsub8[:], in0=gsub_f[:], scalar1=0.8, scalar2=None, op0=ALU.mult)
    V("tensor_tensor", [SETUP], [C2], out=lam_p[:, 0, :], in0=lam_t[:, 0, :], in1=lam_t[:, 1, :], op=ALU.mult)
    V("tensor_tensor", [SETUP], [C2], out=lam_p[:, 1, :], in0=lam_t[:, 2, :], in1=lam_t[:, 3, :], op=ALU.mult)
    V("tensor_reduce", [C2], [C2], out=lam_s[:], in_=lam_p[:], axis=AX.X, op=ALU.add)
    ACT(lam_e[:], lam_s[:], AF.Exp, [C2], [C2])
    V("tensor_tensor", [C2], [C2], out=neglam[:], in0=lam_e[:, 1:2], in1=lam_e[:, 0:1], op=ALU.subtract)
    V("tensor_scalar", [C2], [C2], out=neglam[:], in0=neglam[:], scalar1=-0.2, scalar2=None, op0=ALU.add)
    CR = [CONST, C2, CSEL, CIDB]
    S.barrier()
    if upto == "setup":
        return nc

    WS = [sb("wslot%d" % i, [128, 8, 1024], BF16, R_W + i * 16384) for i in range(3)]
    bWS = [Buf("wslot%d" % i) for i in range(3)]

    def load_w(slot, src_cols):
        S.dma("pool", WS[slot][:], src_cols.rearrange("(c p) n -> p c n", p=128), W=[bWS[slot]])

    XT = [sb("xt%d" % i, [128, 4, 1024], F32, R_A + i * 16384) for i in range(2)]
    bXT = [Buf("xt%d" % i) for i in range(2)]
    XS = sb("xs4", [128, 4, 1024], F32, R_A + 32768)
    bXS = Buf("xs4")
    SQ = sb("sq", [128, 1024], F32, R_A + 49152)
    bSQ = Buf("sq")
    HT = [sb("hT%d" % i, [128, 8, 512], BF16, R_S + i * 8192) for i in range(2)]
    bHT = [Buf("hT%d" % i) for i in range(2)]
    st_ss = sb("st_ss", [128, 64], F32, R_A + 53248)
    st_sq = sb("st_sq", [128, 64], F32, R_A + 53504)
    st_r = sb("st_r", [128, 64], F32, R_A + 53760)
    bST = Buf("stats")

    SQs = [SQ, sb("sq2", [128, 1024], F32, R_A + 57344)]
    bSQs = [bSQ, Buf("sq2")]
    bSTs = [Buf("st%d" % i) for i in range(4)]
    st_cnt = [0]

    def rms_head(src_ap, ngroups, glen, R):
        k = st_cnt[0]
        st_cnt[0] += 1
        sq, bsq = SQs[k % 2], bSQs[k % 2]
        c0 = 16 * (k % 4)
        bst = bSTs[k % 4]
        ACT(sq[:, 0:ngroups * glen], src_ap, AF.Square, R, [bsq])
        V("tensor_reduce", [bsq], [bst], out=st_ss[:, c0:c0 + ngroups],
          in_=sq[:, 0:ngroups * glen].rearrange("p (g d) -> p g d", d=glen), axis=AX.X, op=ALU.add)
        return (c0, ngroups, glen, bst)

    def rms_tail(ctx):
        c0, ngroups, glen, bst = ctx
        ACT(st_sq[:, c0:c0 + ngroups], st_ss[:, c0:c0 + ngroups], AF.Sqrt, [bst] + CR, [bst],
            bias=epsT[:, 0:1], scale=1.0 / glen)
        V("reciprocal", [bst], [bst], out=st_r[:, c0:c0 + ngroups], in_=st_sq[:, c0:c0 + ngroups])
        return st_r[:, c0:c0 + ngroups], bst

    def rms_stats(src_ap, ngroups, glen, R):
        return rms_tail(rms_head(src_ap, ngroups, glen, R))

    load_w(0, w_in[:, 3072:4096])
    load_w(1, w_in[:, 4096:5120])
    KTsb = [sb("ktsb%d" % i, [128, 8, 512], BF16, R_B + i * 8192) for i in range(2)]
    bKTsb = [Buf("ktsb%d" % i) for i in range(2)]
    Vsb = [sb("vsb%d" % i, [128, 8, 4, 129], BF16, R_B + 16384 + i * 8256) for i in range(1)]
    bVsb = [Buf("vsb0")]
    V("memset", [], [bVsb[0]], ap=Vsb[0][:], constant=1.0)
    KNT = sb("knt", [128, 4, 1024], F32, R_W + 32768)
    bKNT = Buf("knt")
    bKTs = [Buf("KVscr", dram=True)] * NSB
    bVs = bKTs

    def load_x(i, src_rows):
        S.dma("sp", XT[i % 2][:], src_rows.rearrange("(b p) d -> p b d", p=128), W=[bXT[i % 2]])

    def qk_norm_block(ps, bps, dst_ap, R_extra, W):
        ctx = rms_head(ps, 8, 64, [bps])

        def tail():
            r_, bst_ = rms_tail(ctx)
            V("tensor_tensor", [bps, bst_], W, out=dst_ap.rearrange("p (g d) -> p g d", d=64),
              in0=ps.rearrange("p (g d) -> p g d", d=64),
              in1=r_.unsqueeze(2).to_broadcast([128, 8, 64]), op=ALU.mult)
        return tail

    def norm_part(xt, bxt):
        prev = None
        for tb in range(4):
            ctx = rms_head(xt[:, tb, :], 1, 1024, [bxt])
            if prev is not None:
                r_, bst_ = rms_tail(prev[1])
                V("tensor_scalar", [bxt, bst_], [bXS], out=XS[:, prev[0], :], in0=xt[:, prev[0], :], scalar1=r_[:, 0:1],
                  scalar2=None, op0=ALU.mult)
            prev = (tb, ctx)
        r_, bst_ = rms_tail(prev[1])
        V("tensor_scalar", [bxt, bst_], [bXS], out=XS[:, prev[0], :], in0=xt[:, prev[0], :], scalar1=r_[:, 0:1],
          scalar2=None, op0=ALU.mult)

    def tx_part(hT, bhT, gcol):
        for c in range(8):
            pt, bpt = (PA[:, 0:512], bPA0) if c % 2 == 0 else (PA[:, 512:1024], bPA1)
            for tb in range(4):
                TR(pt[:, tb * 128:(tb + 1) * 128], XS[:, tb, c * 128:(c + 1) * 128], ident_f[:], [bXS] + CR, [bpt], sig=(tb == 3))
            V("tensor_scalar", [bpt] + CR, [bhT], out=hT[:, c, :], in0=pt, scalar1=gcol[:, c:c + 1], scalar2=None, op0=ALU.mult)

    KB4 = [(PB[:, 0:512], bPB0), (PB[:, 512:1024], bPB1), (PD[:, 0:512], bPD), (PD[:, 512:1024], bPD1)]

    pend = [None]

    def kv_kproj(sbi):
        hT, bhT = HT[sbi % 2], bHT[sbi % 2]
        for tb in range(4):
            for half in range(2):
                ps, bps = KB4[(tb * 2 + half) % 4]
                for c in range(8):
                    MM(ps, hT[:, c, tb * 128:(tb + 1) * 128], WS[0][:, c, half * 512:(half + 1) * 512], c == 0, c == 7,
                       [bhT, bWS[0]], [bps], sig=(c == 7))
                t_ = qk_norm_block(ps, bps, KNT[:, tb, half * 512:(half + 1) * 512], [], [bKNT])
                if pend[0] is not None:
                    pend[0]()
                pend[0] = t_
        pend[0]()
        pend[0] = None

    def kv_vproj(sbi):
        hT, bhT = HT[sbi % 2], bHT[sbi % 2]
        vs, bvs = Vsb[0], bVsb[0]
        for tb in range(4):
            for half in range(2):
                ps, bps = (PC[:, 0:512], bPC0) if half == 0 else (PC[:, 512:1024], bPC1)
                for c in range(8):
                    MM(ps, hT[:, c, tb * 128:(tb + 1) * 128], WS[1][:, c, half * 512:(half + 1) * 512], c == 0, c == 7,
                       [bhT, bWS[1]], [bps], sig=(c == 7))
                ACT(vs[:, half * 4:(half + 1) * 4, tb, 0:128], ps.rearrange("p (h d) -> p h d", d=128), AF.Copy, [bps], [bvs])
        S.dma("sp", Vs[sbi], vs[:].rearrange("p h b d -> p (h b d)"), R=[bvs], W=[bVs[sbi]], join=True)

    def kv_tk(sbi):
        kts, bkts = KTsb[sbi % 2], bKTsb[sbi % 2]
        for h in range(8):
            pt, bpt = (PA[:, 0:512], bPA0) if h % 2 == 0 else (PA[:, 512:1024], bPA1)
            for tb in range(4):
                TR(pt[:, tb * 128:(tb + 1) * 128], KNT[:, tb, h * 128:(h + 1) * 128], ident_f[:], [bKNT] + CR, [bpt], sig=(tb == 3))
            ACT(kts[:, h, :], pt, AF.Copy, [bpt] + CR, [bkts], scale=gqk[:, 1:2])
        S.dma("sp", KTs[sbi], kts[:].rearrange("p h t -> p (h t)"), R=[bkts], W=[bKTs[sbi]], join=True)

    load_x(0, xf[0:512, :])
    load_x(1, xf[512:1024, :])
    norm_part(XT[0], bXT[0])
    tx_part(HT[0], bHT[0], g1c)
    for sbi in range(NSB):
        if sbi + 2 < NSB:
            load_x(sbi + 2, xf[(sbi + 2) * 512:(sbi + 3) * 512, :])
        if sbi + 1 < NSB:
            norm_part(XT[(sbi + 1) % 2], bXT[(sbi + 1) % 2])
        kv_kproj(sbi)
        kv_vproj(sbi)
        if sbi + 1 < NSB:
            tx_part(HT[(sbi + 1) % 2], bHT[(sbi + 1) % 2], g1c)
        kv_tk(sbi)
    S.barrier()
    if upto == "kv":
        return nc

    bhTs = [Buf("hTs", dram=True)] * 4
    bQTs = [Buf("QTs", dram=True)] * 4
    byaTs = [Buf("yaTs", dram=True)] * 4
    bATs = [Buf("ATs", dram=True)] * 4
    bmTs = [Buf("mTs", dram=True)] * 4
    load_w(0, w_in[:, 2048:3072])
    load_w(1, w_in[:, 0:1024])
    load_w(2, w_in[:, 1024:2048])
    QTsb = KTsb
    bQTsb = bKTsb
    def a1_qproj(s):
        hT, bhT = HT[s % 2], bHT[s % 2]
        for tb in range(4):
            for half in range(2):
                ps, bps = KB4[(tb * 2 + half) % 4]
                for c in range(8):
                    MM(ps, hT[:, c, tb * 128:(tb + 1) * 128], WS[0][:, c, half * 512:(half + 1) * 512], c == 0, c == 7,
                       [bhT, bWS[0]], [bps], sig=(c == 7))
                t_ = qk_norm_block(ps, bps, KNQ[:, tb, half * 512:(half + 1) * 512], [], [bKNQ])
                if pend[0] is not None:
                    pend[0]()
                pend[0] = t_
        pend[0]()
        pend[0] = None

    def a1_tq(s):
        qts, bqts = QTsb[s % 2], bQTsb[s % 2]
        for h in range(8):
            pt, bpt = (PC[:, 0:512], bPC0) if h % 2 == 0 else (PC[:, 512:1024], bPC1)
            for tb in range(4):
                TR(pt[:, tb * 128:(tb + 1) * 128], KNQ[:, tb, h * 128:(h + 1) * 128], ident_f[:], [bKNQ] + CR, [bpt], sig=(tb == 3))
            ACT(qts[:, h, :], pt, AF.Copy, [bpt] + CR, [bqts], scale=gqk[:, 0:1])
        S.dma("sp", QTs[s], qts[:].rearrange("p h t -> p (h t)"), R=[bqts], W=[bQTs[s]], join=True)

    KNQ = sb("knq", [128, 4, 1024], F32, R_B + 16384)
    bKNQ = Buf("knq")
    load_x(0, xo[0:512, :])
    load_x(1, xo[512:1024, :])
    norm_part(XT[0], bXT[0])
    tx_part(HT[0], bHT[0], g1c)
    for s in range(4):
        hT, bhT = HT[s % 2], bHT[s % 2]
        S.dma("sp", hTs[s], hT[:].rearrange("p c t -> p (c t)"), R=[bhT], W=[bhTs[s]], join=True)
        if s + 2 < 4:
            load_x(s + 2, xo[(s + 2) * 512:(s + 3) * 512, :])
        if s + 1 < 4:
            norm_part(XT[(s + 1) % 2], bXT[(s + 1) % 2])
        a1_qproj(s)
        if s + 1 < 4:
            tx_part(HT[(s + 1) % 2], bHT[(s + 1) % 2], g1c)
        a1_tq(s)
    S.barrier()
    if upto == "a1":
        return nc
    load_w(0, w_up_a)
    AIN = [sb("ain%d" % i, [128, 8, 512], BF16, R_A + i * 8192) for i in range(4)]
    bAIN = [Buf("ain%d" % i) for i in range(4)]
    VN = sb("vn", [128, 4, 1024], BF16, R_A + 32768)
    bVN = Buf("vn")
    UT = sb("uT", [128, 8, 512], BF16, R_A + 40960)
    bUT = Buf("uT")
    TMP = sb("tmpf", [128, 512], F32, R_A + 55296)
    bTMP = Buf("tmpf")
    TMP2 = sb("tmpf2", [128, 512], F32, R_A + 57344 + 4096)
    bTMP2 = Buf("tmpf2")
    AOUT = [sb("aout%d" % i, [128, 8, 512], BF16, R_B + i * 8192) for i in range(2)]
    bAOUT = [Buf("aout%d" % i) for i in range(2)]

    def load_act(i, src, bsrc):
        S.dma("sp", AIN[i][:].rearrange("p c t -> p (c t)"), src, R=[bsrc], W=[bAIN[i]])

    load_act(0, hTs[0], bhTs[0])
    for s in range(4):
        if s + 1 < 4:
            load_act((s + 1) % 2, hTs[s + 1], bhTs[s + 1])
        hT, bhT = AIN[s % 2], bAIN[s % 2]
        ya, bya = AOUT[s % 2], bAOUT[s % 2]
        for m in range(8):
            ps, bps = (PA[:, 0:512], bPA0) if m % 2 == 0 else (PA[:, 512:1024], bPA1)
            for c in range(8):
                MM(ps, WS[1][:, c, m * 128:(m + 1) * 128], hT[:, c, :], c == 0, c == 7, [bhT, bWS[1]], [bps], sig=(c == 7))
            ACT(UT[:, m, :], ps, AF.Copy, [bps], [bUT])
        for tb in range(4):
            for half in range(2):
                ps, bps = (PB[:, 0:512], bPB0) if half == 0 else (PB[:, 512:1024], bPB1)
                for c in range(8):
                    MM(ps, hT[:, c, tb * 128:(tb + 1) * 128], WS[2][:, c, half * 512:(half + 1) * 512], c == 0, c == 7,
                       [bhT, bWS[2]], [bps], sig=(c == 7))
                r_, bst_ = rms_stats(ps, 4, 128, [bps])
                V("tensor_tensor", [bps, bst_], [bTMP], out=TMP[:].rearrange("p (g d) -> p g d", d=128),
                  in0=ps.rearrange("p (g d) -> p g d", d=128),
                  in1=r_.unsqueeze(2).to_broadcast([128, 4, 128]), op=ALU.mult)
                V("tensor_tensor", [bTMP] + CR, [bVN], out=VN[:, tb, half * 512:(half + 1) * 512], in0=TMP[:],
                  in1=gv_bc[:, half * 512:(half + 1) * 512], op=ALU.mult)
        for g in range(8):
            ps, bps = (PC[:, 0:512], bPC0) if g % 2 == 0 else (PC[:, 512:1024], bPC1)
            for tb in range(4):
                MM(ps[:, tb * 128:(tb + 1) * 128], VN[:, tb, g * 128:(g + 1) * 128], wsT_m[:, g, :], True, True,
                   [bVN] + CR, [bps], sig=(tb == 3))
            V("tensor_tensor", [bps] + CR, [bTMP2], out=TMP2[:].rearrange("p (b t) -> p b t", t=128),
              in0=ps.rearrange("p (b t) -> p b t", t=128),
              in1=bs_t[:, g, :].unsqueeze(1).to_broadcast([128, 4, 128]), op=ALU.add)
            V("tensor_tensor", [bTMP2, bUT], [bya], out=ya[:, g, :], in0=TMP2[:], in1=UT[:, g, :], op=ALU.mult)
        S.dma("sp", yaTs[s], ya[:].rearrange("p c t -> p (c t)"), R=[bya], W=[byaTs[s]], join=True)
    S.barrier()
    if upto == "a2":
        return nc
    load_w(1, w_gate[:, 0:1024])
    SG = sb("sgf", [128, 512], F32, R_A + 49152)
    bSG = Buf("sgf")
    load_act(0, hTs[0], bhTs[0])
    load_act(2, yaTs[0], byaTs[0])
    for s in range(4):
        if s + 1 < 4:
            load_act((s + 1) % 2, hTs[s + 1], bhTs[s + 1])
            load_act(2 + (s + 1) % 2, yaTs[s + 1], byaTs[s + 1])
        hT, bhT = AIN[s % 2], bAIN[s % 2]
        ya, bya = AIN[2 + s % 2], bAIN[2 + s % 2]
        at, bat = AOUT[s % 2], bAOUT[s % 2]
        for m in range(8):
            psu, bpsu = (PA[:, 0:512], bPA0) if m % 2 == 0 else (PA[:, 512:1024], bPA1)
            psg, bpsg = (PB[:, 0:512], bPB0) if m % 2 == 0 else (PB[:, 512:1024], bPB1)
            for c in range(8):
                MM(psg, WS[1][:, c, m * 128:(m + 1) * 128], hT[:, c, :], c == 0, c == 7, [bhT, bWS[1]], [bpsg], sig=(c == 7))
            for c in range(8):
                MM(psu, WS[0][:, c, m * 128:(m + 1) * 128], ya[:, c, :], c == 0, c == 7, [bya, bWS[0]], [bpsu], sig=(c == 7))
            ACT(SG[:], psg, AF.Sigmoid, [bpsg] + CR, [bSG], bias=bgate[:, m:m + 1])
            V("tensor_tensor", [bSG, bpsu], [bat], out=at[:, m, :], in0=SG[:], in1=psu, op=ALU.mult)
        S.dma("sp", ATs[s], at[:].rearrange("p c t -> p (c t)"), R=[bat], W=[bATs[s]], join=True)
    S.barrier()
    if upto == "a3":
        return nc

    YBR = sb("ybraw", [128, 8, 2048], BF16, R_B)
    bYBR = [Buf("ybraw%d" % i) for i in range(4)]
    MK = sb("mk", [128, 16, 512], BF16, R_W)
    bMK = Buf("mk")
    TRI = sb("tri4", [128, 4, 512], BF16, R_W + 16384)
    ABI = sb("abias", [128, NAB], F32, R_W + 20480)
    ATTC = Buf("attc")
    ATTC2 = Buf("attc2")
    S.dma("pool", TRI[:], c_tri4, W=[ATTC2])
    S.dma("sp", ABI[:], c_abias, W=[ATTC])
    load_w(2, w_out)
    NPT = 3
    PT = [sb("pT%d" % i, [128, 2, 512], BF16, R_A + 32768 + i * 2048) for i in range(NPT)]
    bPT = [Buf("pT%d" % i) for i in range(NPT)]
    NKB = 4
    KTb = [sb("ktb%d" % i, [128, 512], BF16, R_A + 4096 + i * 1024) for i in range(NKB)]
    bKTb = [Buf("ktb%d" % i) for i in range(NKB)]
    VB = [sb("vb%d" % i, [128, 4, 129], BF16, R_A + 8192 + i * 1056) for i in range(NKB)]
    bVB = [Buf("vb%d" % i) for i in range(NKB)]
    QKFULL = bool(os.environ.get("KDBG_QKFULL", "0") == "1")
    QB = [sb("qb%d" % i, [128, 2, 512], BF16, R_A + i * 2048) for i in range(2)]
    bQB = [Buf("qb%d" % i) for i in range(2)]
    for i in range(2):
        V("memset", [], [bQB[i]], ap=QB[i][:], constant=0.0)

    def load_q(i, s_, h_):
        S.dma("sp", QB[i][0:64, 0, :], QTs[s_][0:64, h_ * 512:(h_ + 1) * 512], R=[bQTs[s_]], W=[bQB[i]])
        S.dma("sp", QB[i][64:128, 1, :], QTs[s_][64:128, h_ * 512:(h_ + 1) * 512], R=[bQTs[s_]], W=[bQB[i]], join=True)
    RZ = sb("rz", [128, 2, 512], F32, R_A + 16384)
    FT = sb("ft", [128, 2, 512], F32, R_A + 20480)
    bRZ = Buf("rz")
    bFT = Buf("ft")
    ZC = sb("zc", [128, 2, 512], F32, R_A + 24576)
    OC = sb("oc", [128, 2, 512], F32, R_A + 28672)
    bZC = Buf("zc")
    bOC = Buf("oc")
    OACC = [(PC[:, 0:512], bPC0), (PC[:, 512:1024], bPC1)]
    ZACC = [(PD[:, 0:512], bPD), (PD[:, 512:1024], bPD1)]

    loads = [(s, h, t) for s in range(4) for h in range(8) for t in range(KMAX[s])]

    def issue_load(i):
        s, h, t = loads[i]
        S.dma("sp", KTb[i % NKB][:], KTs[t][:, h * 512:(h + 1) * 512], R=[bKTs[t]], W=[bKTb[i % NKB]])
        S.dma("sp", VB[i % NKB][:].rearrange("p b d -> p (b d)"), Vs[t][:, h * 516:(h + 1) * 516], R=[bVs[t]], W=[bVB[i % NKB]])

    li = 0
    issue_load(0)
    issue_load(1)
    qi = 0
    load_q(0, 0, 0)
    for s in range(4):
        for u in range(4):
            for kb in range(4):
                V("tensor_scalar", [ATTC2] + CR, [bMK], eng="pool", out=MK[:, u * 4 + kb, :], in0=TRI[:, kb, :],
                  scalar1=mcoef[:, (s * 4 + u) * 2 + 1:(s * 4 + u) * 2 + 2], scalar2=mcoef[:, (s * 4 + u) * 2:(s * 4 + u) * 2 + 1],
                  op0=ALU.mult, op1=ALU.add)
        for h in range(8):
            qb, bqb = QB[qi % 2], bQB[qi % 2]
            nxt = qi + 1
            if nxt < 32:
                s2, h2 = nxt // 8, nxt % 8
                load_q(nxt % 2, s2, h2)
            qi += 1
            items = [(t, kb) for t in range(KMAX[s]) for kb in range(4)]
            W_ = WH[h]
            nsub = 512 // W_
            nit = len(items)

            def QK(i, li_t):
                t, kb = items[i]
                kt, bkt = KTb[li_t % NKB], bKTb[li_t % NKB]
                st, bst0, bst1 = (PA, bPA0, bPA1) if i % 2 == 0 else (PB, bPB0, bPB1)
                if QKFULL:
                    MM(st[:, 0:512], kt[:, kb * 128:(kb + 1) * 128], qb[:, 0, :], True, True, [bkt, bqb], [bst0])
                    MM(st[:, 512:1024], kt[:, kb * 128:(kb + 1) * 128], qb[:, 1, :], True, True, [bkt, bqb], [bst1], sig=True)
                else:
                    MM(st[:, 0:512], kt[0:64, kb * 128:(kb + 1) * 128], qb[0:64, 0, :], True, True, [bkt, bqb], [bst0])
                    MM(st[:, 512:1024], kt[64:128, kb * 128:(kb + 1) * 128], qb[64:128, 1, :], True, True, [bkt, bqb], [bst1], sig=True)

            li_of = lambda i: li + items[i][0]

            def AVZ(i):
                t_, kb_ = items[i]
                lt_ = li_of(i)
                pt_, bpt_ = PT[i % NPT], bPT[i % NPT]
                vb, bvb = VB[lt_ % NKB], bVB[lt_ % NKB]
                for c in range(2):
                    MM(OACC[c][0], vb[:, kb_, 0:128], pt_[:, c, :], i == 0, i == nit - 1, [bpt_, bvb], [OACC[c][1]])
                for c in range(2):
                    MM(ZACC[c][0], ones_b[:], pt_[:, c, :], i == 0, i == nit - 1, [bpt_] + CR, [ZACC[c][1]], sig=(c == 1))

            QK(0, li_of(0))
            for i, (t, kb) in enumerate(items):
                lt = li_of(i)
                if kb == 0 and lt + 2 < len(loads):
                    issue_load(lt + 2)
                if i + 1 < nit:
                    QK(i + 1, li_of(i + 1))
                st, bst0, bst1 = (PA, bPA0, bPA1) if i % 2 == 0 else (PB, bPB0, bPB1)
                pt, bpt = PT[i % NPT], bPT[i % NPT]
                kbg = 4 * t + kb
                st3 = st[:].rearrange("p (c q) -> p c q", c=2)
                for r in range(nsub):
                    col = ABOFF[(h, s, r)] + kbg
                    ACT(pt[:, :, r * W_:(r + 1) * W_], st3[:, :, r * W_:(r + 1) * W_], AF.Exp, [bst0, bst1, ATTC], [bpt],
                        bias=ABI[:, col:col + 1], scale=0.125, sig=(r == nsub - 1))
                if t >= 4 * s:
                    u = t - 4 * s
                    V("tensor_tensor", [bMK], [bpt], out=pt[:], in0=pt[:],
                      in1=MK[:, u * 4 + kb, :].unsqueeze(1).to_broadcast([128, 2, 512]), op=ALU.mult, sig=True,
                      eng=("pool" if (i % 2 == 1 and os.environ.get("KDBG_MASKENG", "dve") == "mix") else "dve"))
                if i >= 1:
                    AVZ(i - 1)
            AVZ(nit - 1)
            li += KMAX[s]
            for c in range(2):
                ACT(ZC[:, c, :], ZACC[c][0], AF.Copy, [ZACC[c][1]], [bZC], sig=True)
            for c in range(2):
                ACT(OC[:, c, :], OACC[c][0], AF.Copy, [OACC[c][1]], [bOC], sig=True)
            for c in range(2):
                V("reciprocal", [bZC], [bRZ], out=RZ[:, c, :], in_=ZC[:, c, :])
            for c in range(2):
                V("tensor_tensor", [bOC, bRZ], [bFT], out=FT[:, c, :], in0=OC[:, c, :], in1=RZ[:, c, :], op=ALU.mult)
            V("scalar_tensor_tensor", [bFT] + CR, [bYBR[s]], out=YBR[:, h, s * 512:(s + 1) * 512], in0=FT[:, 1, :],
              scalar=neglam[:, 0:1], in1=FT[:, 0, :], op0=ALU.mult, op1=ALU.add)
    S.barrier()
    if upto == "b":
        return nc

    load_w(0, w_up_b)
    load_w(1, w_gate[:, 1024:2048])
    YBT = sb("ybT", [128, 8, 512], BF16, R_A + 32768)
    bYBT = Buf("ybT")
    load_act(0, hTs[0], bhTs[0])
    load_act(2, ATs[0], bATs[0])
    MOUT = [sb("mout%d" % i, [128, 8, 512], BF16, R_A + 40960 + i * 8192) for i in range(1)]
    bMOUT = [Buf("mout0")]
    SQB = [sb("sqb%d" % i, [128, 512], BF16, R_A + 57344 + i * 1024) for i in range(2)]
    bSQB = [Buf("sqb%d" % i) for i in range(2)]
    RSD = [sb("rsd%d" % i, [128, 512], F32, R_A + 59392 + i * 2048) for i in range(2)]
    bRSD = [Buf("rsd%d" % i) for i in range(2)]
    SG2 = sb("sgf2", [128, 512], F32, R_A + 49152)
    TMP3 = sb("tmpf3", [128, 512], F32, R_A + 53248)
    bSG2 = Buf("sgf2")
    bTMP3 = Buf("tmpf3")
    YBT2 = [YBT, sb("ybT1", [128, 8, 512], BF16, R_S)]
    bYBT2 = [bYBT, Buf("ybT1")]

    def c3_norm(s, heads=range(8)):
        ybt, bybt = YBT2[s % 2], bYBT2[s % 2]
        for c in heads:
            raw = YBR[:, c, s * 512:(s + 1) * 512]
            pss, bpss = (PC[:, 0:512], bPC0) if c % 2 == 0 else (PC[:, 512:1024], bPC1)
            V("tensor_tensor", [bYBR[s]], [bSQB[c % 2]], out=SQB[c % 2][:], in0=raw, in1=raw, op=ALU.mult)
            MM(pss, ones_b[:], SQB[c % 2][:], True, True, [bSQB[c % 2]] + CR, [bpss], sig=True)
            ACT(RSD[c % 2][:], pss, AF.Ln, [bpss] + CR, [bRSD[c % 2]], bias=epsT[:, 0:1], scale=1.0 / 128)
            ACT(RSD[c % 2][:], RSD[c % 2][:], AF.Exp, [bRSD[c % 2]], [bRSD[c % 2]], scale=-0.5)
            V("scalar_tensor_tensor", [bYBR[s], bRSD[c % 2]] + CR, [bybt], out=ybt[:, c, :], in0=raw, scalar=gsub8[:, 0:1],
              in1=RSD[c % 2][:], op0=ALU.mult, op1=ALU.mult)

    def c3_mm(s, interleave_next=False):
        ybt, bybt = YBT2[s % 2], bYBT2[s % 2]
        hT, bhT = AIN[s % 2], bAIN[s % 2]
        at, bat = AIN[2 + s % 2], bAIN[2 + s % 2]
        mo, bmo = MOUT[0], bMOUT[0]
        for m in range(8):
            psu, bpsu = (PA[:, 0:512], bPA0) if m % 2 == 0 else (PA[:, 512:1024], bPA1)
            psg, bpsg = (PB[:, 0:512], bPB0) if m % 2 == 0 else (PB[:, 512:1024], bPB1)
            for c in range(8):
                MM(psg, WS[1][:, c, m * 128:(m + 1) * 128], hT[:, c, :], c == 0, c == 7, [bhT, bWS[1]], [bpsg], sig=(c == 7))
            for c in range(8):
                MM(psu, WS[0][:, c, m * 128:(m + 1) * 128], ybt[:, c, :], c == 0, c == 7, [bybt, bWS[0]], [bpsu], sig=(c == 7))
            ACT(SG2[:], psg, AF.Sigmoid, [bpsg] + CR, [bSG2], bias=bgate[:, 8 + m:9 + m])
            V("tensor_tensor", [bSG2, bpsu], [bTMP3], out=TMP3[:], in0=SG2[:], in1=psu, op=ALU.mult)
            V("tensor_tensor", [bTMP3, bat], [bmo], out=mo[:, m, :], in0=TMP3[:], in1=at[:, m, :], op=ALU.add)
            if interleave_next:
                c3_norm(s + 1, heads=[m])
        S.dma("sp", mTs[s], mo[:].rearrange("p c t -> p (c t)"), R=[bmo], W=[bmTs[s]], join=True)

    c3_norm(0)
    for s in range(4):
        if s + 1 < 4:
            load_act((s + 1) % 2, hTs[s + 1], bhTs[s + 1])
            load_act(2 + (s + 1) % 2, ATs[s + 1], bATs[s + 1])
        c3_mm(s, interleave_next=(s + 1 < 4))
    S.barrier()
    if upto == "c3":
        return nc
    X1 = sb("x1", [128, 16, 1024], F32, R_A)
    bX1 = [Buf("x1_%d" % i) for i in range(4)]
    H2T = sb("h2T", [128, 8, 2048], BF16, R_B)
    bH2T = [Buf("h2T_%d" % i) for i in range(4)]
    MIN = [sb("min%d" % i, [128, 8, 512], BF16, R_S + i * 8192) for i in range(2)]
    bMIN = [Buf("min%d" % i) for i in range(2)]
    XS2 = sb("xs2", [128, 4, 1024], F32, R_W)
    bXS2 = Buf("xs2")
    H2F = sb("h2f", [128, 8, 512], F32, R_W + 16384)
    bH2F = Buf("h2f")
    ro = [R_S + 16384]

    def ralloc(name, cols, dt=F32):
        nb = (cols * (4 if dt == F32 else 2) + 31) // 32 * 32
        t = sb(name, [128, cols], dt, ro[0])
        ro[0] += nb
        assert ro[0] <= R_S + 24576, name
        return t

    r_sq = ralloc("r_sq", 1024)
    r_st = ralloc("r_st", 16)
    lg = ralloc("lg", 36)
    r_m = ralloc("r_m", 8)
    r_e = ralloc("r_e", 4)
    r_oh = ralloc("r_oh", 4)
    r_pen = ralloc("r_pen", 4)
    r_em = ralloc("r_em", 32)
    r_oh1 = ralloc("r_oh1", 32)
    r_oh2 = ralloc("r_oh2", 32)
    r_c = ralloc("r_c", 32)
    r_w = ralloc("r_w", 8)
    r_cc = ralloc("r_cc", 64, BF16)
    r_lo = ralloc("r_lo", 32)
    r_c32 = ralloc("r_c32", 64)
    bRT = Buf("rt")
    bCT = Buf("cT")
    S.dma("sp", MIN[0][:].rearrange("p c t -> p (c t)"), mTs[0], R=[bmTs[0]], W=[bMIN[0]])

    def c4_proj(s):
        S.dma("sp", X1[:, s * 4:(s + 1) * 4, :], xo[s * 512:(s + 1) * 512, :].rearrange("(b p) d -> p b d", p=128), W=[bX1[s]])
        if s + 1 < 4:
            S.dma("sp", MIN[(s + 1) % 2][:].rearrange("p c t -> p (c t)"), mTs[s + 1], R=[bmTs[s + 1]], W=[bMIN[(s + 1) % 2]])
        mi, bmi = MIN[s % 2], bMIN[s % 2]
        for tb in range(4):
            for half in range(2):
                ps, bps = (PA[:, 0:512], bPA0) if half == 0 else (PA[:, 512:1024], bPA1)
                for c in range(8):
                    MM(ps, mi[:, c, tb * 128:(tb + 1) * 128], WS[2][:, c, half * 512:(half + 1) * 512], c == 0, c == 7,
                       [bmi, bWS[2]], [bps], sig=(c == 7))
                xv = X1[:, s * 4 + tb, half * 512:(half + 1) * 512]
                V("tensor_tensor", [bps], [bX1[s]], out=xv, in0=xv, in1=ps, op=ALU.add)

    def c4_norm(s):
        for tb in range(4):
            xrow = X1[:, s * 4 + tb, :]
            ACT(r_sq[:], xrow, AF.Square, [bX1[s]], [bRT])
            V("tensor_reduce", [bRT], [bRT], out=r_st[:, 0:1], in_=r_sq[:], axis=AX.X, op=ALU.add)
            ACT(r_st[:, 1:2], r_st[:, 0:1], AF.Sqrt, [bRT] + CR, [bRT], bias=epsT[:, 0:1], scale=1.0 / 1024)
            V("reciprocal", [bRT], [bRT], out=r_st[:, 2:3], in_=r_st[:, 1:2])
            V("tensor_scalar", [bX1[s], bRT], [bXS2], out=XS2[:, tb, :], in0=xrow, scalar1=r_st[:, 2:3], scalar2=None, op0=ALU.mult)

    def c4_tx(s):
        for c in range(8):
            pt, bpt = (PB[:, 0:512], bPB0) if c % 2 == 0 else (PB[:, 512:1024], bPB1)
            for tb in range(4):
                TR(pt[:, tb * 128:(tb + 1) * 128], XS2[:, tb, c * 128:(c + 1) * 128], ident_f[:], [bXS2] + CR, [bpt], sig=(tb == 3))
            V("tensor_scalar", [bpt] + CR, [bH2T[s]], out=H2T[:, c, s * 512:(s + 1) * 512], in0=pt, scalar1=g2c[:, c:c + 1],
              scalar2=None, op0=ALU.mult)
            V("tensor_scalar", [bpt] + CR, [bH2F], out=H2F[:, c, :], in0=pt, scalar1=g2c[:, c:c + 1], scalar2=None, op0=ALU.mult)

    def c4_route(s):
        for tb in range(4):
            for c in range(8):
                MM(PC[:, tb * 36:(tb + 1) * 36], H2F[:, c, tb * 128:(tb + 1) * 128], wr_t[:, c, :], c == 0, c == 7, [bH2F] + CR, [bPC0],
                   sig=(c == 7 and tb == 3))
        B3 = lambda ap, n: ap.unsqueeze(2).to_broadcast([128, 4, n])
        V("tensor_tensor", [bPC0] + CR, [bRT2], out=q_lg[:], in0=PC[:, 0:144].rearrange("p (b e) -> p b e", e=36),
          in1=br_t[:].unsqueeze(1).to_broadcast([128, 4, 36]), op=ALU.add)
        gl = q_lg[:, :, 0:4]
        V("tensor_reduce", [bRT2], [bRT2], out=q_s[:, 0, :], in_=gl, axis=AX.X, op=ALU.max)
        V("tensor_tensor", [bRT2], [bRT2], out=q_ge[:], in0=gl, in1=B3(q_s[:, 0, :], 4), op=ALU.subtract)
        ACT(q_ge[:], q_ge[:], AF.Exp, [bRT2], [bRT2])
        V("tensor_reduce", [bRT2], [bRT2], out=q_s[:, 1, :], in_=q_ge[:], axis=AX.X, op=ALU.add)
        V("reciprocal", [bRT2], [bRT2], out=q_s[:, 2, :], in_=q_s[:, 1, :])
        V("tensor_tensor", [bRT2], [bRT2], out=q_oh[:], in0=gl, in1=B3(q_s[:, 0, :], 4), op=ALU.is_equal)
        V("tensor_scalar", [bRT2], [bRT2], out=q_oh[:], in0=q_oh[:], scalar1=-1.0, scalar2=1e30, op0=ALU.add, op1=ALU.mult)
        V("tensor_tensor", [bRT2], [bRT2], out=q_em[:].rearrange("p b (g e) -> p b g e", e=8),
          in0=q_lg[:, :, 4:36].rearrange("p b (g e) -> p b g e", e=8),
          in1=q_oh[:].unsqueeze(3).to_broadcast([128, 4, 4, 8]), op=ALU.add)
        V("tensor_reduce", [bRT2], [bRT2], out=q_s[:, 3, :], in_=q_em[:], axis=AX.X, op=ALU.max)
        V("tensor_tensor", [bRT2], [bRT2], out=q_o1[:], in0=q_em[:], in1=B3(q_s[:, 3, :], 32), op=ALU.is_equal)
        V("scalar_tensor_tensor", [bRT2], [bRT2], out=q_em[:], in0=q_o1[:], scalar=-1e30, in1=q_em[:], op0=ALU.mult, op1=ALU.add)
        V("tensor_reduce", [bRT2], [bRT2], out=q_s[:, 4, :], in_=q_em[:], axis=AX.X, op=ALU.max)
        V("tensor_tensor", [bRT2], [bRT2], out=q_o2[:], in0=q_em[:], in1=B3(q_s[:, 4, :], 32), op=ALU.is_equal)
        V("tensor_tensor", [bRT2], [bRT2], out=q_s[:, 5, :], in0=q_s[:, 3, :], in1=q_s[:, 4, :], op=ALU.subtract)
        ACT(q_s[:, 6, :], q_s[:, 5, :], AF.Exp, [bRT2], [bRT2], scale=-1.0)
        V("tensor_scalar", [bRT2], [bRT2], out=q_s[:, 6, :], in0=q_s[:, 6, :], scalar1=1.0, scalar2=None, op0=ALU.add)
        V("reciprocal", [bRT2], [bRT2], out=q_s[:, 7, :], in_=q_s[:, 6, :])
        V("tensor_tensor", [bRT2], [bRT2], out=q_s[:, 8, :], in0=q_s[:, 7, :], in1=q_s[:, 2, :], op=ALU.mult)
        V("tensor_tensor", [bRT2], [bRT2], out=q_s[:, 9, :], in0=q_s[:, 2, :], in1=q_s[:, 8, :], op=ALU.subtract)
        V("tensor_tensor", [bRT2], [bRT2], out=q_o1[:], in0=q_o1[:], in1=B3(q_s[:, 8, :], 32), op=ALU.mult)
        V("tensor_tensor", [bRT2], [bRT2], out=q_o2[:], in0=q_o2[:], in1=B3(q_s[:, 9, :], 32), op=ALU.mult)
        V("tensor_tensor", [bRT2], [bRT2], out=q_o1[:], in0=q_o1[:], in1=q_o2[:], op=ALU.add)
        V("tensor_copy", [bRT2], [bRT2], out=q_cb[:], in_=q_o1[:])
        V("tensor_copy", [bRT2], [bRT2], out=q_c32[:, :, 0:32], in_=q_cb[:])
        V("tensor_tensor", [bRT2], [bRT2], out=q_c32[:, :, 32:64], in0=q_o1[:], in1=q_c32[:, :, 0:32], op=ALU.subtract)
        for tb in range(4):
            TR(PD[0:64, 512 + tb * 128:512 + (tb + 1) * 128], q_c32[:, tb, :], ident_f[:], [bRT2] + CR, [bPD1], sig=(tb == 3))
        V("tensor_copy", [bPD1], [bCT], out=cT[:, s * 512:(s + 1) * 512], in_=PD[0:64, 512:1024])

    q_lg = calloc("q_lg", [128, 4, 36], F32)
    q_s = calloc("q_s", [128, 10, 4], F32)
    q_ge = calloc("q_ge", [128, 4, 4], F32)
    q_oh = calloc("q_oh", [128, 4, 4], F32)
    q_em = calloc("q_em", [128, 4, 32], F32)
    q_o1 = calloc("q_o1", [128, 4, 32], F32)
    q_o2 = calloc("q_o2", [128, 4, 32], F32)
    q_cb = calloc("q_cb", [128, 4, 32], BF16)
    q_c32 = calloc("q_c32", [128, 4, 64], F32)
    bRT2 = Buf("rt2")
    c4_proj(0)
    for s in range(4):
        c4_norm(s)
        if s + 1 < 4:
            c4_proj(s + 1)
        c4_tx(s)
        c4_route(s)
    S.barrier()
    if upto == "c4":
        return nc

    EWT = [sb("ew_%d" % i, [128, 6144], BF16, R_W + i * 12288) for i in range(4)]
    EW13 = [t[:, 0:4096].rearrange("p (c f) -> p c f", f=512) for t in EWT]
    EW2 = [t[:, 4096:6144].rearrange("p (f d) -> p f d", d=1024) for t in EWT]
    bEW = [Buf("ew%d" % i) for i in range(4)]
    HID = [sb("hid%d" % i, [128, 2, 512], BF16, R_S + i * 2048) for i in range(4)]
    bHID = [Buf("hid%d" % i) for i in range(4)]
    SA = [sb("sa%d" % i, [128, 512], F32, R_S + 8192 + i * 2048) for i in range(2)]
    bSA = [Buf("sa%d" % i) for i in range(2)]
    TT = [sb("tt%d" % i, [128, 512], F32, R_S + 12288 + i * 2048) for i in range(2)]
    bTT = [Buf("tt%d" % i) for i in range(2)]

    def load_expert(e):
        i = e % 4
        S.dma("pool", EWT[i][:], ewp[e], W=[bEW[i]])

    load_expert(0)
    load_expert(1)
    out_toks = []

    def moe_hid(gp, sbi):
        hcols = slice(sbi * 512, (sbi + 1) * 512)
        for k in range(2):
            e = 2 * gp + k
            wi = e % 4
            hid, bhid = HID[(sbi % 2) * 2 + k], bHID[(sbi % 2) * 2 + k]
            aps = [(PA[:, 0:512], bPA0), (PA[:, 512:1024], bPA1)]
            bps_ = [(PB[:, 0:512], bPB0), (PB[:, 512:1024], bPB1)]
            for fc in range(2):
                for c in range(8):
                    MM(aps[fc][0], EW13[wi][:, c, fc * 128:(fc + 1) * 128], H2T[:, c, hcols], c == 0, c == 7,
                       [bEW[wi], bH2T[sbi]], [aps[fc][1]], sig=(c == 7))
            for fc in range(2):
                for c in range(8):
                    MM(bps_[fc][0], EW13[wi][:, c, 256 + fc * 128:256 + (fc + 1) * 128], H2T[:, c, hcols], c == 0, c == 7,
                       [bEW[wi], bH2T[sbi]], [bps_[fc][1]], sig=(c == 7))
            MM(PD[:, 0:512], sel_t[:, e, :], cT[:, hcols], True, True, [bCT] + CR, [bPD], sig=True)
            for fc in range(2):
                ACT(SA[fc][:], aps[fc][0], AF.Silu, [aps[fc][1]], [bSA[fc]])
                V("tensor_tensor", [bSA[fc], bps_[fc][1]], [bTT[fc]], out=TT[fc][:], in0=SA[fc][:], in1=bps_[fc][0], op=ALU.mult)
                V("tensor_tensor", [bTT[fc], bPD], [bhid], out=hid[:, fc, :], in0=TT[fc][:], in1=PD[:, 0:512], op=ALU.mult)

    def moe_y(gp, sbi):
        for tb in range(4):
            for half in range(2):
                ps, bps = (PC[:, 0:512], bPC0) if half == 0 else (PC[:, 512:1024], bPC1)
                n = 0
                for k in range(2):
                    e = 2 * gp + k
                    wi = e % 4
                    hid, bhid = HID[(sbi % 2) * 2 + k], bHID[(sbi % 2) * 2 + k]
                    for fc in range(2):
                        MM(ps, hid[:, fc, tb * 128:(tb + 1) * 128], EW2[wi][:, fc, half * 512:(half + 1) * 512], n == 0, n == 3,
                           [bhid, bEW[wi]], [bps], sig=(n == 3))
                        n += 1
                xv = X1[:, sbi * 4 + tb, half * 512:(half + 1) * 512]
                V("tensor_tensor", [bps], [bX1[sbi]], out=xv, in0=xv, in1=ps, op=ALU.add)
        if gp == NE // 2 - 1:
            out_toks.append(S.dma("sp", out[sbi * 512:(sbi + 1) * 512, :].rearrange("(b p) d -> p b d", p=128),
                                  X1[:, sbi * 4:(sbi + 1) * 4, :], R=[bX1[sbi]]))

    steps = [(gp, sbi) for gp in range(NE // 2) for sbi in range(4)]
    for i, (gp, sbi) in enumerate(steps):
        moe_hid(gp, sbi)
        if i >= 1:
            moe_y(*steps[i - 1])
        if sbi == 0 and gp + 1 < NE // 2:
            load_expert(2 * gp + 2)
            load_expert(2 * gp + 3)
    moe_y(*steps[-1])
    S.finish(out_toks)
    return nc


_NC_CACHE = {}


def _host_consts(inp):
    f = np.float32
    L = 0
    c = {}
    c["c_ident"] = np.eye(128, dtype=f)
    c["c_g1"] = np.ascontiguousarray(inp["norm1_g"][L].reshape(8, 128).T)
    c["c_g2"] = np.ascontiguousarray(inp["norm2_g"][L].reshape(8, 128).T)
    gq = np.concatenate([inp["q_norm_g"][L], inp["q_norm_g"][L]])
    gk = np.concatenate([inp["k_norm_g"][L], inp["k_norm_g"][L]])
    c["c_gqk"] = np.ascontiguousarray(np.stack([gq, gk], axis=1).astype(f))
    c["c_gv"] = np.ascontiguousarray(np.broadcast_to(inp["v_norm_g"][L].reshape(1, 1024), (128, 1024)))
    c["c_wsT"] = np.ascontiguousarray(inp["w_s"][L].transpose(2, 0, 1))
    c["c_triu"] = np.triu(np.ones((128, 128), f))
    c["c_bs"] = np.ascontiguousarray(np.broadcast_to(inp["b_s"][L][None], (128, 8, 128)))
    c["c_bgate"] = np.ascontiguousarray(inp["b_gate"][L].reshape(16, 128).T)
    c["c_gsub"] = np.ascontiguousarray(inp["sub_norm_g"][L].reshape(128, 1))
    lam = np.stack([inp["lambda_q1"][L], inp["lambda_k1"][L], inp["lambda_q2"][L], inp["lambda_k2"][L]])
    c["c_lam"] = np.ascontiguousarray(np.broadcast_to(lam[None], (128, 4, 64)))
    br = np.concatenate([inp["b_rg"][L], inp["b_re"][L]])
    c["c_br"] = np.ascontiguousarray(np.broadcast_to(br[None], (128, 36)))
    wr = np.concatenate([inp["w_rg"][L], inp["w_re"][L]], axis=1)
    c["c_wr"] = np.ascontiguousarray(wr.reshape(8, 128, 36).transpose(1, 0, 2))
    sel = np.zeros((64, NE, 128), f)
    for e in range(NE):
        sel[e, e, :] = 1.0
        sel[32 + e, e, :] = 1.0
    c["c_sel"] = sel
    k = np.arange(128)[:, None, None]
    kb = np.arange(4)[None, :, None]
    q = np.arange(512)[None, None, :]
    c["c_tri4"] = ((128 * kb + k) <= q).astype(f)
    return c


def _core_tables(j):
    f = np.float32
    sbs = [j, 7 - j, 8 + j, 15 - j]
    mcoef = np.zeros((32,), f)
    for s in range(4):
        for u in range(4):
            t = 4 * s + u
            a, b = (1.0, 0.0) if t < sbs[s] else ((0.0, 1.0) if t == sbs[s] else (0.0, 0.0))
            mcoef[(s * 4 + u) * 2] = a
            mcoef[(s * 4 + u) * 2 + 1] = b
    ab = np.zeros((128, NAB), f)
    p = np.arange(128, dtype=np.float64)
    for h in range(8):
        W = WH[h]
        for s in range(4):
            for r in range(512 // W):
                ref = 512 * sbs[s] + r * W + W / 2
                base = ABOFF[(h, s, r)]
                for kbg in range(4 * KMAX[s]):
                    v = SLOPES[h] * (128 * kbg + p - ref)
                    ab[:, base + kbg] = np.minimum(v, SLOPES[h] * W / 2)
    return sbs, np.ascontiguousarray(np.broadcast_to(mcoef[None], (128, 32))), ab


def kernel(**inputs):
    inp = {k: np.asarray(v) for k, v in inputs.items()}
    x = inp["x"]
    if "nc" not in _NC_CACHE:
        _NC_CACHE["nc"] = build_nc()
    nc = _NC_CACHE["nc"]
    consts = _host_consts(inp)
    shared = {
        "w_in": np.ascontiguousarray(inp["w_in"][0]), "w_gate": np.ascontiguousarray(inp["w_gate"][0]),
        "w_up_a": np.ascontiguousarray(inp["w_up_a"][0]), "w_up_b": np.ascontiguousarray(inp["w_up_b"][0]),
        "w_out": np.ascontiguousarray(inp["w_out"][0]),
    }
    w13 = np.concatenate([inp["w1"][0].reshape(NE, 8, 128, 256), inp["w3"][0].reshape(NE, 8, 128, 256)], axis=3)
    w13 = w13.transpose(0, 2, 1, 3).reshape(NE, 128, 4096)
    w2p = inp["w2"][0].reshape(NE, 2, 128, 1024).transpose(0, 2, 1, 3).reshape(NE, 128, 2048)
    shared["ewp"] = np.ascontiguousarray(np.concatenate([w13, w2p], axis=2))
    shared.update(consts)
    in_maps = []
    own = []
    for c in range(8):
        b, j = c // 4, c % 4
        sbs, mcoef, ab = _core_tables(j)
        xo = np.concatenate([x[b, sb * 512:(sb + 1) * 512] for sb in sbs], axis=0)
        m = dict(shared)
        m["xf"] = np.ascontiguousarray(x[b])
        m["xo"] = np.ascontiguousarray(xo)
        m["c_mcoef"] = mcoef
        m["c_abias"] = ab
        in_maps.append(m)
        own.append((b, sbs))
    res = run_bass_kernel_spmd(nc, in_maps, core_ids=list(range(8)))
    outp = np.empty((2, SEQ, D), np.float32)
    for c in range(8):
        b, sbs = own[c]
        o = np.asarray(res.results[c]["out"])
        for i, sb in enumerate(sbs):
            outp[b, sb * 512:(sb + 1) * 512] = o[i * 512:(i + 1) * 512]
    return outp
```

```python
import os
import numpy as np
import concourse.bass as bass
import concourse.mybir as mybir
from concourse.bass_utils import run_bass_kernel_spmd

F32 = mybir.dt.float32
BF16 = mybir.dt.bfloat16
AF = mybir.ActivationFunctionType
ALU = mybir.AluOpType
AX = mybir.AxisListType

D = 1024
SEQ = 8192
NSB = 16
KMAX = [4, 8, 12, 16]
EPS = 1e-6
NE = 32
WH = [256, 512, 512, 512, 512, 512, 512, 512]
SLOPES = [2.0 ** (-(h + 1)) for h in range(8)]


def abias_layout():
    off = {}
    n = 0
    for h in range(8):
        nsub = 512 // WH[h]
        for s in range(4):
            for r in range(nsub):
                off[(h, s, r)] = n
                n += 4 * KMAX[s]
    return off, n


ABOFF, NAB = abias_layout()


class Buf:
    def __init__(self, name, dram=False):
        self.name = name
        self.dram = dram
        self.w = {}
        self.r = {}
        self.dsem = None
        self.dcnt = 0


class Sched:
    CE = ("pe", "act", "dve", "pool")

    def __init__(self, nc):
        self.nc = nc
        self.E = {"pe": nc.tensor, "act": nc.scalar, "dve": nc.vector, "pool": nc.gpsimd, "sp": nc.sync}
        self.sem = {k: nc.alloc_semaphore(name="cs_" + k) for k in self.CE}
        self.n = {k: 0 for k in self.CE}
        self.last = {k: None for k in self.CE}
        self.sig = {k: [] for k in self.CE}
        self.seen = {k: {} for k in self.E}
        self.dsems = []

    def _value_for(self, F, i):
        sl = self.sig[F]
        lo, hi = 0, len(sl)
        while lo < hi:
            mid = (lo + hi) // 2
            if sl[mid] >= i:
                hi = mid
            else:
                lo = mid + 1
        if lo < len(sl):
            return lo + 1
        self._signal_last(F)
        return len(sl)

    def _signal_last(self, F):
        if self.sig[F] and self.sig[F][-1] == self.n[F]:
            return
        self.last[F].then_inc(self.sem[F], 1)
        self.sig[F].append(self.n[F])

    def _wait(self, E, tok):
        if tok is None:
            return
        if tok[0] == "dma":
            _, sem, val, key = tok
            if self.seen[E].get(key, 0) >= val:
                return
            self.E[E].wait_ge(sem, val)
            self.seen[E][key] = val
            return
        F, i = tok
        if F == E and E == "pe":
            return
        val = self._value_for(F, i)
        if self.seen[E].get(F, 0) >= val:
            return
        self.E[E].wait_ge(self.sem[F], val)
        self.seen[E][F] = val

    def _deps(self, E, R, W, join=False):
        for b in R:
            for t in list(b.w.values()):
                self._wait(E, t)
        for b in W:
            if not join:
                for t in list(b.w.values()):
                    self._wait(E, t)
            for t in list(b.r.values()):
                self._wait(E, t)

    def op(self, E, fn, R=(), W=(), sig=False):
        self._deps(E, R, W)
        ins = fn()
        self.n[E] += 1
        self.last[E] = ins
        tok = (E, self.n[E])
        for b in R:
            b.r[E] = tok
        for b in W:
            b.w = {E: tok}
            b.r = {}
        if sig:
            self._signal_last(E)
        return ins

    def dma(self, q, out, in_, R=(), W=(), join=False):
        self._deps(q, R, W, join=join)
        sb = W[0] if W else R[0]
        if W and W[0].dram and R:
            sb = R[0]
        if q == "pool":
            self.nsw = getattr(self, "nsw", 0) + 1
            sb = Buf("sw%d_%s" % (self.nsw, sb.name))
            assert not join
        if sb.dsem is None:
            sb.dsem = self.nc.alloc_semaphore(name="ds_" + sb.name)
            self.dsems.append(sb)
        sb.dcnt += 16
        self.E[q].dma_start(out=out, in_=in_).then_inc(sb.dsem, 16)
        tok = ("dma", sb.dsem, sb.dcnt, sb.name)
        for b in R:
            b.r[("dma", sb.name)] = tok
        for b in W:
            if join:
                b.w[("dma", sb.name)] = tok
            else:
                b.w = {("dma", sb.name): tok}
                b.r = {}
        return tok

    def barrier(self):
        for F in self.CE:
            if self.last[F] is not None:
                self._signal_last(F)
        for E in self.E:
            for F in self.CE:
                if self.last[F] is None or F == E:
                    continue
                val = len(self.sig[F])
                if self.seen[E].get(F, 0) < val:
                    self.E[E].wait_ge(self.sem[F], val)
                    self.seen[E][F] = val
            for sb in self.dsems:
                if self.seen[E].get(sb.name, 0) < sb.dcnt:
                    self.E[E].wait_ge(sb.dsem, sb.dcnt)
                    self.seen[E][sb.name] = sb.dcnt

    def finish(self, toks):
        for t in toks:
            self._wait("sp", t)


def build_nc(upto=None, debug=False):
    nc = bass.Bass("TRN2", target_bir_lowering=False)
    S = Sched(nc)

    def din(name, shape, dt=F32):
        return nc.dram_tensor(name, list(shape), dt, kind="ExternalInput").ap()

    xf = din("xf", [SEQ, D])
    xo = din("xo", [2048, D])
    w_in = din("w_in", [D, 5120])
    w_gate = din("w_gate", [D, 2048])
    w_up_a = din("w_up_a", [D, D])
    w_up_b = din("w_up_b", [D, D])
    w_out = din("w_out", [D, D])
    ewp = din("ewp", [NE, 128, 6144])
    c_ident = din("c_ident", [128, 128])
    c_g1 = din("c_g1", [128, 8])
    c_g2 = din("c_g2", [128, 8])
    c_gqk = din("c_gqk", [128, 2])
    c_gv = din("c_gv", [128, 1024])
    c_wsT = din("c_wsT", [128, 8, 128])
    c_triu = din("c_triu", [128, 128])
    c_bs = din("c_bs", [128, 8, 128])
    c_bgate = din("c_bgate", [128, 16])
    c_gsub = din("c_gsub", [128, 1])
    c_lam = din("c_lam", [128, 4, 64])
    c_br = din("c_br", [128, 36])
    c_wr = din("c_wr", [128, 8, 36])
    c_sel = din("c_sel", [64, NE, 128])
    c_tri4 = din("c_tri4", [128, 4, 512])
    c_mcoef = din("c_mcoef", [128, 32])
    c_abias = din("c_abias", [128, NAB])
    out = nc.dram_tensor("out", [2048, D], F32, kind="ExternalOutput").ap()

    def dscr(name, shape, dt=BF16):
        return nc.dram_tensor(name, list(shape), dt, kind=("ExternalOutput" if debug else "Internal")).ap()

    hTs = dscr("hTs", [4, 128, 8 * 512])
    QTs = dscr("QTs", [4, 128, 8 * 512])
    yaTs = dscr("yaTs", [4, 128, 8 * 512])
    ATs = dscr("ATs", [4, 128, 8 * 512])
    mTs = dscr("mTs", [4, 128, 8 * 512])
    KTs = dscr("KTs", [NSB, 128, 8 * 512])
    Vs = dscr("Vs", [NSB, 128, 8 * 4 * 129])

    def sb(name, shape, dt, off):
        return nc.alloc_sbuf_tensor_at(name, list(shape), dt, offset=off)

    BASE = 16512
    R_A = BASE
    R_B = R_A + 65536
    R_W = R_B + 32768
    R_C = R_W + 49152
    R_S = R_C + 32768

    co = [R_C]

    def calloc(name, shape, dt):
        nbytes = int(np.prod(shape[1:])) * (4 if dt == F32 else 2)
        nbytes = (nbytes + 31) // 32 * 32
        t = sb(name, shape, dt, co[0])
        co[0] += nbytes
        assert co[0] <= R_S, name
        return t

    ident_f = calloc("ident_f", [128, 128], F32)
    ident_b = calloc("ident_b", [128, 128], BF16)
    g1c = calloc("g1c", [128, 8], F32)
    g2c = calloc("g2c", [128, 8], F32)
    gqk = calloc("gqk", [128, 2], F32)
    gv_bc = calloc("gv_bc", [128, 1024], F32)
    wsT_m = calloc("wsT_m", [128, 8, 128], BF16)
    bs_t = calloc("bs_t", [128, 8, 128], F32)
    bgate = calloc("bgate", [128, 16], F32)
    gsub8 = calloc("gsub8", [128, 1], F32)
    ones_b = calloc("ones_b", [128, 128], BF16)
    br_t = calloc("br_t", [128, 36], F32)
    wr_t = calloc("wr_t", [128, 8, 36], F32)
    sel_t = calloc("sel_t", [64, NE, 128], BF16)
    mcoef = calloc("mcoef", [128, 32], F32)
    zeros_b = calloc("zeros_b", [128, 640], BF16)
    epsT = calloc("epsT", [128, 1], F32)
    neglam = calloc("neglam", [128, 1], F32)
    cT = calloc("cT", [64, 2048], BF16)
    CONST = Buf("const")

    PA = nc.alloc_psum_tensor("PA", [128, 1024], F32)
    PB = nc.alloc_psum_tensor("PB", [128, 1024], F32)
    PC = nc.alloc_psum_tensor("PC", [128, 1024], F32)
    PD = nc.alloc_psum_tensor("PD", [128, 1024], F32)
    bPA0, bPA1, bPB0, bPB1, bPC0, bPC1, bPD, bPD1 = [Buf("ps%d" % i) for i in range(8)]

    def MM(out_, lhsT, rhs, start, stop, R, W, sig=False, **kw):
        return S.op("pe", lambda: nc.tensor.matmul(out_, lhsT=lhsT, rhs=rhs, start=start, stop=stop, **kw), R, W, sig)

    def TR(out_, in_, ident, R, W, sig=False):
        return S.op("pe", lambda: nc.tensor.transpose(out_, in_, ident), R, W, sig)

    def ACT(out_, in_, func, R, W, bias=None, scale=None, sig=False):
        kw = {}
        if bias is not None:
            kw["bias"] = bias
        if scale is not None:
            kw["scale"] = scale
        return S.op("act", lambda: nc.scalar.activation(out=out_, in_=in_, func=func, **kw), R, W, sig)

    def V(name, R, W, sig=False, eng="dve", **kw):
        e = nc.vector if eng == "dve" else nc.gpsimd
        return S.op(eng, lambda: getattr(e, name)(**kw), R, W, sig)

    lam_t = sb("lam_t", [128, 4, 64], F32, R_A)
    lam_p = sb("lam_p", [128, 2, 64], F32, R_A + 1024)
    lam_s = sb("lam_s", [128, 2], F32, R_A + 1536)
    lam_e = sb("lam_e", [128, 2], F32, R_A + 1568)
    wsT_f = sb("wsT_f", [128, 8, 128], F32, R_A + 2048)
    triu_f = sb("triu_f", [128, 128], F32, R_A + 6144)
    gsub_f = sb("gsub_f", [128, 1], F32, R_A + 6656)
    SETUP = Buf("setup")
    for dst, src in [(ident_f, c_ident), (g1c, c_g1), (g2c, c_g2), (gqk, c_gqk), (gv_bc, c_gv), (bs_t, c_bs),
                     (bgate, c_bgate), (br_t, c_br), (wr_t, c_wr), (mcoef, c_mcoef)]:
        S.dma("sp", dst[:], src, W=[CONST], join=True)
    for dst, src in [(lam_t, c_lam), (wsT_f, c_wsT), (triu_f, c_triu), (gsub_f, c_gsub)]:
        S.dma("sp", dst[:], src, W=[SETUP], join=True)
    CSEL = Buf("csel")
    CIDB = Buf("cidb")
    S.dma("pool", sel_t[:], c_sel, W=[CSEL])
    S.dma("pool", ident_b[:], c_ident, W=[CIDB])
    C2 = Buf("const2")
    V("memset", [], [C2], ap=zeros_b[:], constant=0.0)
    V("memset", [], [C2], ap=epsT[:], constant=EPS)
    V("memset", [], [C2], ap=ones_b[:], constant=1.0)
    V("tensor_tensor", [SETUP], [C2], out=wsT_m[:], in0=wsT_f[:], in1=triu_f[:].unsqueeze(1).to_broadcast([128, 8, 128]), op=ALU.mult)
    V("tensor_scalar", [SETUP], [C2], out=gsub8[:], in0=gsub_f[:], scalar1=0.8, scalar2=None, op0=ALU.mult)
    V("tensor_tensor", [SETUP], [C2], out=lam_p[:, 0, :], in0=lam_t[:, 0, :], in1=lam_t[:, 1, :], op=ALU.mult)
    V("tensor_tensor", [SETUP], [C2], out=lam_p[:, 1, :], in0=lam_t[:, 2, :], in1=lam_t[:, 3, :], op=ALU.mult)
    V("tensor_reduce", [C2], [C2], out=lam_s[:], in_=lam_p[:], axis=AX.X, op=ALU.add)
    ACT(lam_e[:], lam_s[:], AF.Exp, [C2], [C2])
    V("tensor_tensor", [C2], [C2], out=neglam[:], in0=lam_e[:, 1:2], in1=lam_e[:, 0:1], op=ALU.subtract)
    V("tensor_scalar", [C2], [C2], out=neglam[:], in0=neglam[:], scalar1=-0.2, scalar2=None, op0=ALU.add)
    CR = [CONST, C2, CSEL, CIDB]
    S.barrier()
    if upto == "setup":
        return nc

    WS = [sb("wslot%d" % i, [128, 8, 1024], BF16, R_W + i * 16384) for i in range(3)]
    bWS = [Buf("wslot%d" % i) for i in range(3)]

    def load_w(slot, src_cols):
        S.dma("pool", WS[slot][:], src_cols.rearrange("(c p) n -> p c n", p=128), W=[bWS[slot]])

    XT = [sb("xt%d" % i, [128, 4, 1024], F32, R_A + i * 16384) for i in range(2)]
    bXT = [Buf("xt%d" % i) for i in range(2)]
    XS = sb("xs4", [128, 4, 1024], F32, R_A + 32768)
    bXS = Buf("xs4")
    SQ = sb("sq", [128, 1024], F32, R_A + 49152)
    bSQ = Buf("sq")
    HT = [sb("hT%d" % i, [128, 8, 512], BF16, R_S + i * 8192) for i in range(2)]
    bHT = [Buf("hT%d" % i) for i in range(2)]
    st_ss = sb("st_ss", [128, 64], F32, R_A + 53248)
    st_sq = sb("st_sq", [128, 64], F32, R_A + 53504)
    st_r = sb("st_r", [128, 64], F32, R_A + 53760)
    bST = Buf("stats")

    SQs = [SQ, sb("sq2", [128, 1024], F32, R_A + 57344)]
    bSQs = [bSQ, Buf("sq2")]
    bSTs = [Buf("st%d" % i) for i in range(4)]
    st_cnt = [0]

    def rms_head(src_ap, ngroups, glen, R):
        k = st_cnt[0]
        st_cnt[0] += 1
        sq, bsq = SQs[k % 2], bSQs[k % 2]
        c0 = 16 * (k % 4)
        bst = bSTs[k % 4]
        ACT(sq[:, 0:ngroups * glen], src_ap, AF.Square, R, [bsq])
        V("tensor_reduce", [bsq], [bst], out=st_ss[:, c0:c0 + ngroups],
          in_=sq[:, 0:ngroups * glen].rearrange("p (g d) -> p g d", d=glen), axis=AX.X, op=ALU.add)
        return (c0, ngroups, glen, bst)

    def rms_tail(ctx):
        c0, ngroups, glen, bst = ctx
        ACT(st_sq[:, c0:c0 + ngroups], st_ss[:, c0:c0 + ngroups], AF.Sqrt, [bst] + CR, [bst],
            bias=epsT[:, 0:1], scale=1.0 / glen)
        V("reciprocal", [bst], [bst], out=st_r[:, c0:c0 + ngroups], in_=st_sq[:, c0:c0 + ngroups])
        return st_r[:, c0:c0 + ngroups], bst

    def rms_stats(src_ap, ngroups, glen, R):
        return rms_tail(rms_head(src_ap, ngroups, glen, R))

    load_w(0, w_in[:, 3072:4096])
    load_w(1, w_in[:, 4096:5120])
    KTsb = [sb("ktsb%d" % i, [128, 8, 512], BF16, R_B + i * 8192) for i in range(2)]
    bKTsb = [Buf("ktsb%d" % i) for i in range(2)]
    Vsb = [sb("vsb%d" % i, [128, 8, 4, 129], BF16, R_B + 16384 + i * 8256) for i in range(1)]
    bVsb = [Buf("vsb0")]
    V("memset", [], [bVsb[0]], ap=Vsb[0][:], constant=1.0)
    KNT = sb("knt", [128, 4, 1024], F32, R_W + 32768)
    bKNT = Buf("knt")
    bKTs = [Buf("KVscr", dram=True)] * NSB
    bVs = bKTs

    def load_x(i, src_rows):
        S.dma("sp", XT[i % 2][:], src_rows.rearrange("(b p) d -> p b d", p=128), W=[bXT[i % 2]])

    def qk_norm_block(ps, bps, dst_ap, R_extra, W):
        ctx = rms_head(ps, 8, 64, [bps])

        def tail():
            r_, bst_ = rms_tail(ctx)
            V("tensor_tensor", [bps, bst_], W, out=dst_ap.rearrange("p (g d) -> p g d", d=64),
              in0=ps.rearrange("p (g d) -> p g d", d=64),
              in1=r_.unsqueeze(2).to_broadcast([128, 8, 64]), op=ALU.mult)
        return tail

    def norm_part(xt, bxt):
        prev = None
        for tb in range(4):
            ctx = rms_head(xt[:, tb, :], 1, 1024, [bxt])
            if prev is not None:
                r_, bst_ = rms_tail(prev[1])
                V("tensor_scalar", [bxt, bst_], [bXS], out=XS[:, prev[0], :], in0=xt[:, prev[0], :], scalar1=r_[:, 0:1],
                  scalar2=None, op0=ALU.mult)
            prev = (tb, ctx)
        r_, bst_ = rms_tail(prev[1])
        V("tensor_scalar", [bxt, bst_], [bXS], out=XS[:, prev[0], :], in0=xt[:, prev[0], :], scalar1=r_[:, 0:1],
          scalar2=None, op0=ALU.mult)

    def tx_part(hT, bhT, gcol):
        for c in range(8):
            pt, bpt = (PA[:, 0:512], bPA0) if c % 2 == 0 else (PA[:, 512:1024], bPA1)
            for tb in range(4):
                TR(pt[:, tb * 128:(tb + 1) * 128], XS[:, tb, c * 128:(c + 1) * 128], ident_f[:], [bXS] + CR, [bpt], sig=(tb == 3))
            V("tensor_scalar", [bpt] + CR, [bhT], out=hT[:, c, :], in0=pt, scalar1=gcol[:, c:c + 1], scalar2=None, op0=ALU.mult)

    KB4 = [(PB[:, 0:512], bPB0), (PB[:, 512:1024], bPB1), (PD[:, 0:512], bPD), (PD[:, 512:1024], bPD1)]

    pend = [None]

    def kv_kproj(sbi):
        hT, bhT = HT[sbi % 2], bHT[sbi % 2]
        for tb in range(4):
            for half in range(2):
                ps, bps = KB4[(tb * 2 + half) % 4]
                for c in range(8):
                    MM(ps, hT[:, c, tb * 128:(tb + 1) * 128], WS[0][:, c, half * 512:(half + 1) * 512], c == 0, c == 7,
                       [bhT, bWS[0]], [bps], sig=(c == 7))
                t_ = qk_norm_block(ps, bps, KNT[:, tb, half * 512:(half + 1) * 512], [], [bKNT])
                if pend[0] is not None:
                    pend[0]()
                pend[0] = t_
        pend[0]()
        pend[0] = None

    def kv_vproj(sbi):
        hT, bhT = HT[sbi % 2], bHT[sbi % 2]
        vs, bvs = Vsb[0], bVsb[0]
        for tb in range(4):
            for half in range(2):
                ps, bps = (PC[:, 0:512], bPC0) if half == 0 else (PC[:, 512:1024], bPC1)
                for c in range(8):
                    MM(ps, hT[:, c, tb * 128:(tb + 1) * 128], WS[1][:, c, half * 512:(half + 1) * 512], c == 0, c == 7,
                       [bhT, bWS[1]], [bps], sig=(c == 7))
                ACT(vs[:, half * 4:(half + 1) * 4, tb, 0:128], ps.rearrange("p (h d) -> p h d", d=128), AF.Copy, [bps], [bvs])
        S.dma("sp", Vs[sbi], vs[:].rearrange("p h b d -> p (h b d)"), R=[bvs], W=[bVs[sbi]], join=True)

    def kv_tk(sbi):
        kts, bkts = KTsb[sbi % 2], bKTsb[sbi % 2]
        for h in range(8):
            pt, bpt = (PA[:, 0:512], bPA0) if h % 2 == 0 else (PA[:, 512:1024], bPA1)
            for tb in range(4):
                TR(pt[:, tb * 128:(tb + 1) * 128], KNT[:, tb, h * 128:(h + 1) * 128], ident_f[:], [bKNT] + CR, [bpt], sig=(tb == 3))
            ACT(kts[:, h, :], pt, AF.Copy, [bpt] + CR, [bkts], scale=gqk[:, 1:2])
        S.dma("sp", KTs[sbi], kts[:].rearrange("p h t -> p (h t)"), R=[bkts], W=[bKTs[sbi]], join=True)

    load_x(0, xf[0:512, :])
    load_x(1, xf[512:1024, :])
    norm_part(XT[0], bXT[0])
    tx_part(HT[0], bHT[0], g1c)
    for sbi in range(NSB):
        if sbi + 2 < NSB:
            load_x(sbi + 2, xf[(sbi + 2) * 512:(sbi + 3) * 512, :])
        if sbi + 1 < NSB:
            norm_part(XT[(sbi + 1) % 2], bXT[(sbi + 1) % 2])
        kv_kproj(sbi)
        kv_vproj(sbi)
        if sbi + 1 < NSB:
            tx_part(HT[(sbi + 1) % 2], bHT[(sbi + 1) % 2], g1c)
        kv_tk(sbi)
    S.barrier()
    if upto == "kv":
        return nc

    bhTs = [Buf("hTs", dram=True)] * 4
    bQTs = [Buf("QTs", dram=True)] * 4
    byaTs = [Buf("yaTs", dram=True)] * 4
    bATs = [Buf("ATs", dram=True)] * 4
    bmTs = [Buf("mTs", dram=True)] * 4
    load_w(0, w_in[:, 2048:3072])
    load_w(1, w_in[:, 0:1024])
    load_w(2, w_in[:, 1024:2048])
    QTsb = KTsb
    bQTsb = bKTsb
    def a1_qproj(s):
        hT, bhT = HT[s % 2], bHT[s % 2]
        for tb in range(4):
            for half in range(2):
                ps, bps = KB4[(tb * 2 + half) % 4]
                for c in range(8):
                    MM(ps, hT[:, c, tb * 128:(tb + 1) * 128], WS[0][:, c, half * 512:(half + 1) * 512], c == 0, c == 7,
                       [bhT, bWS[0]], [bps], sig=(c == 7))
                t_ = qk_norm_block(ps, bps, KNQ[:, tb, half * 512:(half + 1) * 512], [], [bKNQ])
                if pend[0] is not None:
                    pend[0]()
                pend[0] = t_
        pend[0]()
        pend[0] = None

    def a1_tq(s):
        qts, bqts = QTsb[s % 2], bQTsb[s % 2]
        for h in range(8):
            pt, bpt = (PC[:, 0:512], bPC0) if h % 2 == 0 else (PC[:, 512:1024], bPC1)
            for tb in range(4):
                TR(pt[:, tb * 128:(tb + 1) * 128], KNQ[:, tb, h * 128:(h + 1) * 128], ident_f[:], [bKNQ] + CR, [bpt], sig=(tb == 3))
            ACT(qts[:, h, :], pt, AF.Copy, [bpt] + CR, [bqts], scale=gqk[:, 0:1])
        S.dma("sp", QTs[s], qts[:].rearrange("p h t -> p (h t)"), R=[bqts], W=[bQTs[s]], join=True)

    KNQ = sb("knq", [128, 4, 1024], F32, R_B + 16384)
    bKNQ = Buf("knq")
    load_x(0, xo[0:512, :])
    load_x(1, xo[512:1024, :])
    norm_part(XT[0], bXT[0])
    tx_part(HT[0], bHT[0], g1c)
    for s in range(4):
        hT, bhT = HT[s % 2], bHT[s % 2]
        S.dma("sp", hTs[s], hT[:].rearrange("p c t -> p (c t)"), R=[bhT], W=[bhTs[s]], join=True)
        if s + 2 < 4:
            load_x(s + 2, xo[(s + 2) * 512:(s + 3) * 512, :])
        if s + 1 < 4:
            norm_part(XT[(s + 1) % 2], bXT[(s + 1) % 2])
        a1_qproj(s)
        if s + 1 < 4:
            tx_part(HT[(s + 1) % 2], bHT[(s + 1) % 2], g1c)
        a1_tq(s)
    S.barrier()
    if upto == "a1":
        return nc
    load_w(0, w_up_a)
    WS3 = sb("wslot3", [128, 8, 1024], BF16, R_B + 16384)
    bWS3 = Buf("wslot3")
    S.dma("pool", WS3[:], w_gate[:, 0:1024].rearrange("(c p) n -> p c n", p=128), W=[bWS3])
    AIN = [sb("ain%d" % i, [128, 8, 512], BF16, R_A + i * 8192) for i in range(4)]
    bAIN = [Buf("ain%d" % i) for i in range(4)]
    VN = sb("vn", [128, 4, 1024], BF16, R_A + 32768)
    bVN = Buf("vn")
    UT = sb("uT", [128, 8, 512], BF16, R_A + 40960)
    bUT = Buf("uT")
    TMP = sb("tmpf", [128, 512], F32, R_A + 55296)
    bTMP = Buf("tmpf")
    TMP2 = sb("tmpf2", [128, 512], F32, R_A + 57344 + 4096)
    bTMP2 = Buf("tmpf2")
    AOUT = [sb("aout%d" % i, [128, 8, 512], BF16, R_B + i * 8192) for i in range(2)]
    bAOUT = [Buf("aout%d" % i) for i in range(2)]

    def load_act(i, src, bsrc):
        S.dma("sp", AIN[i][:].rearrange("p c t -> p (c t)"), src, R=[bsrc], W=[bAIN[i]])

    load_act(0, hTs[0], bhTs[0])
    for s in range(4):
        if s + 1 < 4:
            load_act((s + 1) % 2, hTs[s + 1], bhTs[s + 1])
        hT, bhT = AIN[s % 2], bAIN[s % 2]
        ya, bya = AOUT[s % 2], bAOUT[s % 2]
        for m in range(8):
            ps, bps = (PA[:, 0:512], bPA0) if m % 2 == 0 else (PA[:, 512:1024], bPA1)
            for c in range(8):
                MM(ps, WS[1][:, c, m * 128:(m + 1) * 128], hT[:, c, :], c == 0, c == 7, [bhT, bWS[1]], [bps], sig=(c == 7))
            ACT(UT[:, m, :], ps, AF.Copy, [bps], [bUT])
        for tb in range(4):
            for half in range(2):
                ps, bps = (PB[:, 0:512], bPB0) if half == 0 else (PB[:, 512:1024], bPB1)
                for c in range(8):
                    MM(ps, hT[:, c, tb * 128:(tb + 1) * 128], WS[2][:, c, half * 512:(half + 1) * 512], c == 0, c == 7,
                       [bhT, bWS[2]], [bps], sig=(c == 7))
                r_, bst_ = rms_stats(ps, 4, 128, [bps])
                V("tensor_tensor", [bps, bst_], [bTMP], out=TMP[:].rearrange("p (g d) -> p g d", d=128),
                  in0=ps.rearrange("p (g d) -> p g d", d=128),
                  in1=r_.unsqueeze(2).to_broadcast([128, 4, 128]), op=ALU.mult)
                V("tensor_tensor", [bTMP] + CR, [bVN], out=VN[:, tb, half * 512:(half + 1) * 512], in0=TMP[:],
                  in1=gv_bc[:, half * 512:(half + 1) * 512], op=ALU.mult)
        for g in range(8):
            ps, bps = (PC[:, 0:512], bPC0) if g % 2 == 0 else (PC[:, 512:1024], bPC1)
            for tb in range(4):
                MM(ps[:, tb * 128:(tb + 1) * 128], VN[:, tb, g * 128:(g + 1) * 128], wsT_m[:, g, :], True, True,
                   [bVN] + CR, [bps], sig=(tb == 3))
            V("tensor_tensor", [bps] + CR, [bTMP2], out=TMP2[:].rearrange("p (b t) -> p b t", t=128),
              in0=ps.rearrange("p (b t) -> p b t", t=128),
              in1=bs_t[:, g, :].unsqueeze(1).to_broadcast([128, 4, 128]), op=ALU.add)
            V("tensor_tensor", [bTMP2, bUT], [bya], out=ya[:, g, :], in0=TMP2[:], in1=UT[:, g, :], op=ALU.mult)
        S.dma("sp", yaTs[s], ya[:].rearrange("p c t -> p (c t)"), R=[bya], W=[byaTs[s]], join=True)
    S.barrier()
    if upto == "a2":
        return nc
    SG = sb("sgf", [128, 512], F32, R_A + 49152)
    bSG = Buf("sgf")
    load_act(0, hTs[0], bhTs[0])
    load_act(2, yaTs[0], byaTs[0])
    for s in range(4):
        if s + 1 < 4:
            load_act((s + 1) % 2, hTs[s + 1], bhTs[s + 1])
            load_act(2 + (s + 1) % 2, yaTs[s + 1], byaTs[s + 1])
        hT, bhT = AIN[s % 2], bAIN[s % 2]
        ya, bya = AIN[2 + s % 2], bAIN[2 + s % 2]
        at, bat = AOUT[s % 2], bAOUT[s % 2]
        for m in range(8):
            psu, bpsu = (PA[:, 0:512], bPA0) if m % 2 == 0 else (PA[:, 512:1024], bPA1)
            psg, bpsg = (PB[:, 0:512], bPB0) if m % 2 == 0 else (PB[:, 512:1024], bPB1)
            for c in range(8):
                MM(psg, WS3[:, c, m * 128:(m + 1) * 128], hT[:, c, :], c == 0, c == 7, [bhT, bWS3], [bpsg], sig=(c == 7))
            for c in range(8):
                MM(psu, WS[0][:, c, m * 128:(m + 1) * 128], ya[:, c, :], c == 0, c == 7, [bya, bWS[0]], [bpsu], sig=(c == 7))
            ACT(SG[:], psg, AF.Sigmoid, [bpsg] + CR, [bSG], bias=bgate[:, m:m + 1])
            V("tensor_tensor", [bSG, bpsu], [bat], out=at[:, m, :], in0=SG[:], in1=psu, op=ALU.mult)
        S.dma("sp", ATs[s], at[:].rearrange("p c t -> p (c t)"), R=[bat], W=[bATs[s]], join=True)
    S.barrier()
    if upto == "a3":
        return nc

    YBR = sb("ybraw", [128, 8, 2048], BF16, R_B)
    bYBR = [Buf("ybraw%d" % i) for i in range(4)]
    MK = sb("mk", [128, 16, 512], BF16, R_W)
    bMK = Buf("mk")
    TRI = sb("tri4", [128, 4, 512], BF16, R_W + 16384)
    ABI = sb("abias", [128, NAB], F32, R_W + 20480)
    ATTC = Buf("attc")
    ATTC2 = Buf("attc2")
    S.dma("pool", TRI[:], c_tri4, W=[ATTC2])
    S.dma("sp", ABI[:], c_abias, W=[ATTC])
    load_w(2, w_out)
    NPT = 3
    PT = [sb("pT%d" % i, [128, 2, 512], BF16, R_A + 32768 + i * 2048) for i in range(NPT)]
    bPT = [Buf("pT%d" % i) for i in range(NPT)]
    NKB = 4
    KTb = [sb("ktb%d" % i, [128, 512], BF16, R_A + 4096 + i * 1024) for i in range(NKB)]
    bKTb = [Buf("ktb%d" % i) for i in range(NKB)]
    VB = [sb("vb%d" % i, [128, 4, 129], BF16, R_A + 8192 + i * 1056) for i in range(NKB)]
    bVB = [Buf("vb%d" % i) for i in range(NKB)]
    QKFULL = bool(os.environ.get("KDBG_QKFULL", "0") == "1")
    QB = [sb("qb%d" % i, [128, 2, 512], BF16, R_A + i * 2048) for i in range(2)]
    bQB = [Buf("qb%d" % i) for i in range(2)]
    for i in range(2):
        V("memset", [], [bQB[i]], ap=QB[i][:], constant=0.0)

    def load_q(i, s_, h_):
        S.dma("sp", QB[i][0:64, 0, :], QTs[s_][0:64, h_ * 512:(h_ + 1) * 512], R=[bQTs[s_]], W=[bQB[i]])
        S.dma("sp", QB[i][64:128, 1, :], QTs[s_][64:128, h_ * 512:(h_ + 1) * 512], R=[bQTs[s_]], W=[bQB[i]], join=True)
    RZ = sb("rz", [128, 2, 512], F32, R_A + 16384)
    FT = sb("ft", [128, 2, 512], F32, R_A + 20480)
    bRZ = Buf("rz")
    bFT = Buf("ft")
    ZC = sb("zc", [128, 2, 512], F32, R_A + 24576)
    OC = sb("oc", [128, 2, 512], F32, R_A + 28672)
    bZC = Buf("zc")
    bOC = Buf("oc")
    OACC = [(PC[:, 0:512], bPC0), (PC[:, 512:1024], bPC1)]
    ZACC = [(PD[:, 0:512], bPD), (PD[:, 512:1024], bPD1)]

    loads = [(s, h, t) for s in range(4) for h in range(8) for t in range(KMAX[s])]

    def issue_load(i):
        s, h, t = loads[i]
        S.dma("sp", KTb[i % NKB][:], KTs[t][:, h * 512:(h + 1) * 512], R=[bKTs[t]], W=[bKTb[i % NKB]])
        S.dma("sp", VB[i % NKB][:].rearrange("p b d -> p (b d)"), Vs[t][:, h * 516:(h + 1) * 516], R=[bVs[t]], W=[bVB[i % NKB]])

    li = 0
    issue_load(0)
    issue_load(1)
    qi = 0
    load_q(0, 0, 0)
    for s in range(4):
        for u in range(4):
            for kb in range(4):
                V("tensor_scalar", [ATTC2] + CR, [bMK], eng="pool", out=MK[:, u * 4 + kb, :], in0=TRI[:, kb, :],
                  scalar1=mcoef[:, (s * 4 + u) * 2 + 1:(s * 4 + u) * 2 + 2], scalar2=mcoef[:, (s * 4 + u) * 2:(s * 4 + u) * 2 + 1],
                  op0=ALU.mult, op1=ALU.add)
        for h in range(8):
            qb, bqb = QB[qi % 2], bQB[qi % 2]
            nxt = qi + 1
            if nxt < 32:
                s2, h2 = nxt // 8, nxt % 8
                load_q(nxt % 2, s2, h2)
            qi += 1
            items = [(t, kb) for t in range(KMAX[s]) for kb in range(4)]
            W_ = WH[h]
            nsub = 512 // W_
            nit = len(items)

            def QK(i, li_t):
                t, kb = items[i]
                kt, bkt = KTb[li_t % NKB], bKTb[li_t % NKB]
                st, bst0, bst1 = (PA, bPA0, bPA1) if i % 2 == 0 else (PB, bPB0, bPB1)
                if QKFULL:
                    MM(st[:, 0:512], kt[:, kb * 128:(kb + 1) * 128], qb[:, 0, :], True, True, [bkt, bqb], [bst0])
                    MM(st[:, 512:1024], kt[:, kb * 128:(kb + 1) * 128], qb[:, 1, :], True, True, [bkt, bqb], [bst1], sig=True)
                else:
                    MM(st[:, 0:512], kt[0:64, kb * 128:(kb + 1) * 128], qb[0:64, 0, :], True, True, [bkt, bqb], [bst0])
                    MM(st[:, 512:1024], kt[64:128, kb * 128:(kb + 1) * 128], qb[64:128, 1, :], True, True, [bkt, bqb], [bst1], sig=True)

            li_of = lambda i: li + items[i][0]

            def AVZ(i):
                t_, kb_ = items[i]
                lt_ = li_of(i)
                pt_, bpt_ = PT[i % NPT], bPT[i % NPT]
                vb, bvb = VB[lt_ % NKB], bVB[lt_ % NKB]
                for c in range(2):
                    MM(OACC[c][0], vb[:, kb_, 0:128], pt_[:, c, :], i == 0, i == nit - 1, [bpt_, bvb], [OACC[c][1]])
                for c in range(2):
                    MM(ZACC[c][0], ones_b[:], pt_[:, c, :], i == 0, i == nit - 1, [bpt_] + CR, [ZACC[c][1]], sig=(c == 1))

            QK(0, li_of(0))
            for i, (t, kb) in enumerate(items):
                lt = li_of(i)
                if kb == 0 and lt + 2 < len(loads):
                    issue_load(lt + 2)
                if i + 1 < nit:
                    QK(i + 1, li_of(i + 1))
                st, bst0, bst1 = (PA, bPA0, bPA1) if i % 2 == 0 else (PB, bPB0, bPB1)
                pt, bpt = PT[i % NPT], bPT[i % NPT]
                kbg = 4 * t + kb
                st3 = st[:].rearrange("p (c q) -> p c q", c=2)
                for r in range(nsub):
                    col = ABOFF[(h, s, r)] + kbg
                    ACT(pt[:, :, r * W_:(r + 1) * W_], st3[:, :, r * W_:(r + 1) * W_], AF.Exp, [bst0, bst1, ATTC], [bpt],
                        bias=ABI[:, col:col + 1], scale=0.125, sig=(r == nsub - 1))
                if t >= 4 * s:
                    u = t - 4 * s
                    V("tensor_tensor", [bMK], [bpt], out=pt[:], in0=pt[:],
                      in1=MK[:, u * 4 + kb, :].unsqueeze(1).to_broadcast([128, 2, 512]), op=ALU.mult, sig=True,
                      eng=("pool" if (i % 2 == 1 and os.environ.get("KDBG_MASKENG", "dve") == "mix") else "dve"))
                if i >= 1:
                    AVZ(i - 1)
            AVZ(nit - 1)
            li += KMAX[s]
            for c in range(2):
                ACT(ZC[:, c, :], ZACC[c][0], AF.Copy, [ZACC[c][1]], [bZC], sig=True)
            for c in range(2):
                ACT(OC[:, c, :], OACC[c][0], AF.Copy, [OACC[c][1]], [bOC], sig=True)
            for c in range(2):
                V("reciprocal", [bZC], [bRZ], out=RZ[:, c, :], in_=ZC[:, c, :])
            for c in range(2):
                V("tensor_tensor", [bOC, bRZ], [bFT], out=FT[:, c, :], in0=OC[:, c, :], in1=RZ[:, c, :], op=ALU.mult)
            V("scalar_tensor_tensor", [bFT] + CR, [bYBR[s]], out=YBR[:, h, s * 512:(s + 1) * 512], in0=FT[:, 1, :],
              scalar=neglam[:, 0:1], in1=FT[:, 0, :], op0=ALU.mult, op1=ALU.add)
    S.barrier()
    if upto == "b":
        return nc

    load_w(0, w_up_b)
    load_w(1, w_gate[:, 1024:2048])
    YBT = sb("ybT", [128, 8, 512], BF16, R_A + 32768)
    bYBT = Buf("ybT")
    load_act(0, hTs[0], bhTs[0])
    load_act(2, ATs[0], bATs[0])
    MOUT = [sb("mout%d" % i, [128, 8, 512], BF16, R_A + 40960 + i * 8192) for i in range(1)]
    bMOUT = [Buf("mout0")]
    SQB = [sb("sqb%d" % i, [128, 512], BF16, R_A + 57344 + i * 1024) for i in range(2)]
    bSQB = [Buf("sqb%d" % i) for i in range(2)]
    RSD = [sb("rsd%d" % i, [128, 512], F32, R_A + 59392 + i * 2048) for i in range(2)]
    bRSD = [Buf("rsd%d" % i) for i in range(2)]
    SG2 = sb("sgf2", [128, 512], F32, R_A + 49152)
    TMP3 = sb("tmpf3", [128, 512], F32, R_A + 53248)
    bSG2 = Buf("sgf2")
    bTMP3 = Buf("tmpf3")
    YBT2 = [YBT, sb("ybT1", [128, 8, 512], BF16, R_S)]
    bYBT2 = [bYBT, Buf("ybT1")]

    def c3_norm(s, heads=range(8)):
        ybt, bybt = YBT2[s % 2], bYBT2[s % 2]
        for c in heads:
            raw = YBR[:, c, s * 512:(s + 1) * 512]
            pss, bpss = (PC[:, 0:512], bPC0) if c % 2 == 0 else (PC[:, 512:1024], bPC1)
            ACT(SQB[c % 2][:], raw, AF.Square, [bYBR[s]], [bSQB[c % 2]])
            MM(pss, ones_b[:], SQB[c % 2][:], True, True, [bSQB[c % 2]] + CR, [bpss], sig=True)
            ACT(RSD[c % 2][:], pss, AF.Ln, [bpss] + CR, [bRSD[c % 2]], bias=epsT[:, 0:1], scale=1.0 / 128)
            ACT(RSD[c % 2][:], RSD[c % 2][:], AF.Exp, [bRSD[c % 2]], [bRSD[c % 2]], scale=-0.5)
            V("scalar_tensor_tensor", [bYBR[s], bRSD[c % 2]] + CR, [bybt], out=ybt[:, c, :], in0=raw, scalar=gsub8[:, 0:1],
              in1=RSD[c % 2][:], op0=ALU.mult, op1=ALU.mult)

    def c3_mm(s, interleave_next=False):
        ybt, bybt = YBT2[s % 2], bYBT2[s % 2]
        hT, bhT = AIN[s % 2], bAIN[s % 2]
        at, bat = AIN[2 + s % 2], bAIN[2 + s % 2]
        mo, bmo = MOUT[0], bMOUT[0]
        for m in range(8):
            psu, bpsu = (PA[:, 0:512], bPA0) if m % 2 == 0 else (PA[:, 512:1024], bPA1)
            psg, bpsg = (PB[:, 0:512], bPB0) if m % 2 == 0 else (PB[:, 512:1024], bPB1)
            for c in range(8):
                MM(psg, WS[1][:, c, m * 128:(m + 1) * 128], hT[:, c, :], c == 0, c == 7, [bhT, bWS[1]], [bpsg], sig=(c == 7))
            for c in range(8):
                MM(psu, WS[0][:, c, m * 128:(m + 1) * 128], ybt[:, c, :], c == 0, c == 7, [bybt, bWS[0]], [bpsu], sig=(c == 7))
            ACT(SG2[:], psg, AF.Sigmoid, [bpsg] + CR, [bSG2], bias=bgate[:, 8 + m:9 + m])
            V("tensor_tensor", [bSG2, bpsu], [bTMP3], out=TMP3[:], in0=SG2[:], in1=psu, op=ALU.mult)
            V("tensor_tensor", [bTMP3, bat], [bmo], out=mo[:, m, :], in0=TMP3[:], in1=at[:, m, :], op=ALU.add)
            if interleave_next:
                c3_norm(s + 1, heads=[m])
        S.dma("sp", mTs[s], mo[:].rearrange("p c t -> p (c t)"), R=[bmo], W=[bmTs[s]], join=True)

    c3_norm(0)
    for s in range(4):
        if s + 1 < 4:
            load_act((s + 1) % 2, hTs[s + 1], bhTs[s + 1])
            load_act(2 + (s + 1) % 2, ATs[s + 1], bATs[s + 1])
        c3_mm(s, interleave_next=(s + 1 < 4))
    S.barrier()
    if upto == "c3":
        return nc
    X1 = sb("x1", [128, 16, 1024], F32, R_A)
    bX1 = [Buf("x1_%d" % i) for i in range(4)]
    H2T = sb("h2T", [128, 8, 2048], BF16, R_B)
    bH2T = [Buf("h2T_%d" % i) for i in range(4)]
    MIN = [sb("min%d" % i, [128, 8, 512], BF16, R_S + i * 8192) for i in range(2)]
    bMIN = [Buf("min%d" % i) for i in range(2)]
    XS2 = sb("xs2", [128, 4, 1024], F32, R_W)
    bXS2 = Buf("xs2")
    H2F = sb("h2f", [128, 8, 512], F32, R_W + 16384)
    bH2F = Buf("h2f")
    ro = [R_S + 16384]

    def ralloc(name, cols, dt=F32):
        nb = (cols * (4 if dt == F32 else 2) + 31) // 32 * 32
        t = sb(name, [128, cols], dt, ro[0])
        ro[0] += nb
        assert ro[0] <= R_S + 24576, name
        return t

    r_sq = ralloc("r_sq", 1024)
    r_st = ralloc("r_st", 16)
    lg = ralloc("lg", 36)
    r_m = ralloc("r_m", 8)
    r_e = ralloc("r_e", 4)
    r_oh = ralloc("r_oh", 4)
    r_pen = ralloc("r_pen", 4)
    r_em = ralloc("r_em", 32)
    r_oh1 = ralloc("r_oh1", 32)
    r_oh2 = ralloc("r_oh2", 32)
    r_c = ralloc("r_c", 32)
    r_w = ralloc("r_w", 8)
    r_cc = ralloc("r_cc", 64, BF16)
    r_lo = ralloc("r_lo", 32)
    r_c32 = ralloc("r_c32", 64)
    bRT = Buf("rt")
    bCT = Buf("cT")
    S.dma("sp", MIN[0][:].rearrange("p c t -> p (c t)"), mTs[0], R=[bmTs[0]], W=[bMIN[0]])

    def c4_proj(s):
        S.dma("sp", X1[:, s * 4:(s + 1) * 4, :], xo[s * 512:(s + 1) * 512, :].rearrange("(b p) d -> p b d", p=128), W=[bX1[s]])
        if s + 1 < 4:
            S.dma("sp", MIN[(s + 1) % 2][:].rearrange("p c t -> p (c t)"), mTs[s + 1], R=[bmTs[s + 1]], W=[bMIN[(s + 1) % 2]])
        mi, bmi = MIN[s % 2], bMIN[s % 2]
        for tb in range(4):
            for half in range(2):
                ps, bps = (PA[:, 0:512], bPA0) if half == 0 else (PA[:, 512:1024], bPA1)
                for c in range(8):
                    MM(ps, mi[:, c, tb * 128:(tb + 1) * 128], WS[2][:, c, half * 512:(half + 1) * 512], c == 0, c == 7,
                       [bmi, bWS[2]], [bps], sig=(c == 7))
                xv = X1[:, s * 4 + tb, half * 512:(half + 1) * 512]
                V("tensor_tensor", [bps], [bX1[s]], out=xv, in0=xv, in1=ps, op=ALU.add)

    def c4_norm(s):
        for tb in range(4):
            xrow = X1[:, s * 4 + tb, :]
            ACT(r_sq[:], xrow, AF.Square, [bX1[s]], [bRT])
            V("tensor_reduce", [bRT], [bRT], out=r_st[:, 0:1], in_=r_sq[:], axis=AX.X, op=ALU.add)
            ACT(r_st[:, 1:2], r_st[:, 0:1], AF.Sqrt, [bRT] + CR, [bRT], bias=epsT[:, 0:1], scale=1.0 / 1024)
            V("reciprocal", [bRT], [bRT], out=r_st[:, 2:3], in_=r_st[:, 1:2])
            V("tensor_scalar", [bX1[s], bRT], [bXS2], out=XS2[:, tb, :], in0=xrow, scalar1=r_st[:, 2:3], scalar2=None, op0=ALU.mult)

    def c4_tx(s):
        for c in range(8):
            pt, bpt = (PB[:, 0:512], bPB0) if c % 2 == 0 else (PB[:, 512:1024], bPB1)
            for tb in range(4):
                TR(pt[:, tb * 128:(tb + 1) * 128], XS2[:, tb, c * 128:(c + 1) * 128], ident_f[:], [bXS2] + CR, [bpt], sig=(tb == 3))
            V("tensor_scalar", [bpt] + CR, [bH2T[s]], out=H2T[:, c, s * 512:(s + 1) * 512], in0=pt, scalar1=g2c[:, c:c + 1],
              scalar2=None, op0=ALU.mult)
            V("tensor_scalar", [bpt] + CR, [bH2F], out=H2F[:, c, :], in0=pt, scalar1=g2c[:, c:c + 1], scalar2=None, op0=ALU.mult)

    def c4_route(s):
        for tb in range(4):
            for c in range(8):
                MM(PC[:, tb * 36:(tb + 1) * 36], H2F[:, c, tb * 128:(tb + 1) * 128], wr_t[:, c, :], c == 0, c == 7, [bH2F] + CR, [bPC0],
                   sig=(c == 7 and tb == 3))
        B3 = lambda ap, n: ap.unsqueeze(2).to_broadcast([128, 4, n])
        V("tensor_tensor", [bPC0] + CR, [bRT2], out=q_lg[:], in0=PC[:, 0:144].rearrange("p (b e) -> p b e", e=36),
          in1=br_t[:].unsqueeze(1).to_broadcast([128, 4, 36]), op=ALU.add)
        gl = q_lg[:, :, 0:4]
        V("tensor_reduce", [bRT2], [bRT2], out=q_s[:, 0, :], in_=gl, axis=AX.X, op=ALU.max)
        V("tensor_tensor", [bRT2], [bRT2], out=q_ge[:], in0=gl, in1=B3(q_s[:, 0, :], 4), op=ALU.subtract)
        ACT(q_ge[:], q_ge[:], AF.Exp, [bRT2], [bRT2])
        V("tensor_reduce", [bRT2], [bRT2], out=q_s[:, 1, :], in_=q_ge[:], axis=AX.X, op=ALU.add)
        V("reciprocal", [bRT2], [bRT2], out=q_s[:, 2, :], in_=q_s[:, 1, :])
        V("tensor_tensor", [bRT2], [bRT2], out=q_oh[:], in0=gl, in1=B3(q_s[:, 0, :], 4), op=ALU.is_equal)
        V("tensor_scalar", [bRT2], [bRT2], out=q_oh[:], in0=q_oh[:], scalar1=-1.0, scalar2=1e30, op0=ALU.add, op1=ALU.mult)
        V("tensor_tensor", [bRT2], [bRT2], out=q_em[:].rearrange("p b (g e) -> p b g e", e=8),
          in0=q_lg[:, :, 4:36].rearrange("p b (g e) -> p b g e", e=8),
          in1=q_oh[:].unsqueeze(3).to_broadcast([128, 4, 4, 8]), op=ALU.add)
        V("tensor_reduce", [bRT2], [bRT2], out=q_s[:, 3, :], in_=q_em[:], axis=AX.X, op=ALU.max)
        V("tensor_tensor", [bRT2], [bRT2], out=q_o1[:], in0=q_em[:], in1=B3(q_s[:, 3, :], 32), op=ALU.is_equal)
        V("scalar_tensor_tensor", [bRT2], [bRT2], out=q_em[:], in0=q_o1[:], scalar=-1e30, in1=q_em[:], op0=ALU.mult, op1=ALU.add)
        V("tensor_reduce", [bRT2], [bRT2], out=q_s[:, 4, :], in_=q_em[:], axis=AX.X, op=ALU.max)
        V("tensor_tensor", [bRT2], [bRT2], out=q_o2[:], in0=q_em[:], in1=B3(q_s[:, 4, :], 32), op=ALU.is_equal)
        V("tensor_tensor", [bRT2], [bRT2], out=q_s[:, 5, :], in0=q_s[:, 3, :], in1=q_s[:, 4, :], op=ALU.subtract)
        ACT(q_s[:, 6, :], q_s[:, 5, :], AF.Exp, [bRT2], [bRT2], scale=-1.0)
        V("tensor_scalar", [bRT2], [bRT2], out=q_s[:, 6, :], in0=q_s[:, 6, :], scalar1=1.0, scalar2=None, op0=ALU.add)
        V("reciprocal", [bRT2], [bRT2], out=q_s[:, 7, :], in_=q_s[:, 6, :])
        V("tensor_tensor", [bRT2], [bRT2], out=q_s[:, 8, :], in0=q_s[:, 7, :], in1=q_s[:, 2, :], op=ALU.mult)
        V("tensor_tensor", [bRT2], [bRT2], out=q_s[:, 9, :], in0=q_s[:, 2, :], in1=q_s[:, 8, :], op=ALU.subtract)
        V("tensor_tensor", [bRT2], [bRT2], out=q_o1[:], in0=q_o1[:], in1=B3(q_s[:, 8, :], 32), op=ALU.mult)
        V("tensor_tensor", [bRT2], [bRT2], out=q_o2[:], in0=q_o2[:], in1=B3(q_s[:, 9, :], 32), op=ALU.mult)
        V("tensor_tensor", [bRT2], [bRT2], out=q_o1[:], in0=q_o1[:], in1=q_o2[:], op=ALU.add)
        V("tensor_copy", [bRT2], [bRT2], out=q_cb[:], in_=q_o1[:])
        V("tensor_copy", [bRT2], [bRT2], out=q_c32[:, :, 0:32], in_=q_cb[:])
        V("tensor_tensor", [bRT2], [bRT2], out=q_c32[:, :, 32:64], in0=q_o1[:], in1=q_c32[:, :, 0:32], op=ALU.subtract)
        for tb in range(4):
            TR(PD[0:64, 512 + tb * 128:512 + (tb + 1) * 128], q_c32[:, tb, :], ident_f[:], [bRT2] + CR, [bPD1], sig=(tb == 3))
        V("tensor_copy", [bPD1], [bCT], out=cT[:, s * 512:(s + 1) * 512], in_=PD[0:64, 512:1024])

    q_lg = calloc("q_lg", [128, 4, 36], F32)
    q_s = calloc("q_s", [128, 10, 4], F32)
    q_ge = calloc("q_ge", [128, 4, 4], F32)
    q_oh = calloc("q_oh", [128, 4, 4], F32)
    q_em = calloc("q_em", [128, 4, 32], F32)
    q_o1 = calloc("q_o1", [128, 4, 32], F32)
    q_o2 = calloc("q_o2", [128, 4, 32], F32)
    q_cb = calloc("q_cb", [128, 4, 32], BF16)
    q_c32 = calloc("q_c32", [128, 4, 64], F32)
    bRT2 = Buf("rt2")
    c4_proj(0)
    for s in range(4):
        c4_norm(s)
        if s + 1 < 4:
            c4_proj(s + 1)
        c4_tx(s)
        c4_route(s)
    S.barrier()
    if upto == "c4":
        return nc

    EWT = [sb("ew_%d" % i, [128, 6144], BF16, R_W + i * 12288) for i in range(4)]
    EW13 = [t[:, 0:4096].rearrange("p (c f) -> p c f", f=512) for t in EWT]
    EW2 = [t[:, 4096:6144].rearrange("p (f d) -> p f d", d=1024) for t in EWT]
    bEW = [Buf("ew%d" % i) for i in range(4)]
    HID = [sb("hid%d" % i, [128, 2, 512], BF16, R_S + i * 2048) for i in range(4)]
    bHID = [Buf("hid%d" % i) for i in range(4)]
    SA = [sb("sa%d" % i, [128, 512], F32, R_S + 8192 + i * 2048) for i in range(2)]
    bSA = [Buf("sa%d" % i) for i in range(2)]
    TT = [sb("tt%d" % i, [128, 512], F32, R_S + 12288 + i * 2048) for i in range(2)]
    bTT = [Buf("tt%d" % i) for i in range(2)]

    def load_expert(e):
        i = e % 4
        S.dma("pool", EWT[i][:], ewp[e], W=[bEW[i]])

    load_expert(0)
    load_expert(1)
    out_toks = []

    def moe_hid(gp, sbi):
        hcols = slice(sbi * 512, (sbi + 1) * 512)
        for k in range(2):
            e = 2 * gp + k
            wi = e % 4
            hid, bhid = HID[(sbi % 2) * 2 + k], bHID[(sbi % 2) * 2 + k]
            aps = [(PA[:, 0:512], bPA0), (PA[:, 512:1024], bPA1)]
            bps_ = [(PB[:, 0:512], bPB0), (PB[:, 512:1024], bPB1)]
            for fc in range(2):
                for c in range(8):
                    MM(aps[fc][0], EW13[wi][:, c, fc * 128:(fc + 1) * 128], H2T[:, c, hcols], c == 0, c == 7,
                       [bEW[wi], bH2T[sbi]], [aps[fc][1]], sig=(c == 7))
            for fc in range(2):
                for c in range(8):
                    MM(bps_[fc][0], EW13[wi][:, c, 256 + fc * 128:256 + (fc + 1) * 128], H2T[:, c, hcols], c == 0, c == 7,
                       [bEW[wi], bH2T[sbi]], [bps_[fc][1]], sig=(c == 7))
            MM(PD[:, 0:512], sel_t[:, e, :], cT[:, hcols], True, True, [bCT] + CR, [bPD], sig=True)
            for fc in range(2):
                ACT(SA[fc][:], aps[fc][0], AF.Silu, [aps[fc][1]], [bSA[fc]])
                V("tensor_tensor", [bSA[fc], bps_[fc][1]], [bTT[fc]], out=TT[fc][:], in0=SA[fc][:], in1=bps_[fc][0], op=ALU.mult)
                V("tensor_tensor", [bTT[fc], bPD], [bhid], out=hid[:, fc, :], in0=TT[fc][:], in1=PD[:, 0:512], op=ALU.mult)

    def moe_y(gp, sbi):
        for tb in range(4):
            for half in range(2):
                ps, bps = (PC[:, 0:512], bPC0) if half == 0 else (PC[:, 512:1024], bPC1)
                n = 0
                for k in range(2):
                    e = 2 * gp + k
                    wi = e % 4
                    hid, bhid = HID[(sbi % 2) * 2 + k], bHID[(sbi % 2) * 2 + k]
                    for fc in range(2):
                        MM(ps, hid[:, fc, tb * 128:(tb + 1) * 128], EW2[wi][:, fc, half * 512:(half + 1) * 512], n == 0, n == 3,
                           [bhid, bEW[wi]], [bps], sig=(n == 3))
                        n += 1
                xv = X1[:, sbi * 4 + tb, half * 512:(half + 1) * 512]
                V("tensor_tensor", [bps], [bX1[sbi]], out=xv, in0=xv, in1=ps, op=ALU.add)
        if gp == NE // 2 - 1:
            out_toks.append(S.dma("sp", out[sbi * 512:(sbi + 1) * 512, :].rearrange("(b p) d -> p b d", p=128),
                                  X1[:, sbi * 4:(sbi + 1) * 4, :], R=[bX1[sbi]]))

    steps = [(gp, sbi) for gp in range(NE // 2) for sbi in range(4)]
    for i, (gp, sbi) in enumerate(steps):
        moe_hid(gp, sbi)
        if i >= 1:
            moe_y(*steps[i - 1])
        if sbi == 0 and gp + 1 < NE // 2:
            load_expert(2 * gp + 2)
            load_expert(2 * gp + 3)
    moe_y(*steps[-1])
    S.finish(out_toks)
    return nc


_NC_CACHE = {}


def _host_consts(inp):
    f = np.float32
    L = 0
    c = {}
    c["c_ident"] = np.eye(128, dtype=f)
    c["c_g1"] = np.ascontiguousarray(inp["norm1_g"][L].reshape(8, 128).T)
    c["c_g2"] = np.ascontiguousarray(inp["norm2_g"][L].reshape(8, 128).T)
    gq = np.concatenate([inp["q_norm_g"][L], inp["q_norm_g"][L]])
    gk = np.concatenate([inp["k_norm_g"][L], inp["k_norm_g"][L]])
    c["c_gqk"] = np.ascontiguousarray(np.stack([gq, gk], axis=1).astype(f))
    c["c_gv"] = np.ascontiguousarray(np.broadcast_to(inp["v_norm_g"][L].reshape(1, 1024), (128, 1024)))
    c["c_wsT"] = np.ascontiguousarray(inp["w_s"][L].transpose(2, 0, 1))
    c["c_triu"] = np.triu(np.ones((128, 128), f))
    c["c_bs"] = np.ascontiguousarray(np.broadcast_to(inp["b_s"][L][None], (128, 8, 128)))
    c["c_bgate"] = np.ascontiguousarray(inp["b_gate"][L].reshape(16, 128).T)
    c["c_gsub"] = np.ascontiguousarray(inp["sub_norm_g"][L].reshape(128, 1))
    lam = np.stack([inp["lambda_q1"][L], inp["lambda_k1"][L], inp["lambda_q2"][L], inp["lambda_k2"][L]])
    c["c_lam"] = np.ascontiguousarray(np.broadcast_to(lam[None], (128, 4, 64)))
    br = np.concatenate([inp["b_rg"][L], inp["b_re"][L]])
    c["c_br"] = np.ascontiguousarray(np.broadcast_to(br[None], (128, 36)))
    wr = np.concatenate([inp["w_rg"][L], inp["w_re"][L]], axis=1)
    c["c_wr"] = np.ascontiguousarray(wr.reshape(8, 128, 36).transpose(1, 0, 2))
    sel = np.zeros((64, NE, 128), f)
    for e in range(NE):
        sel[e, e, :] = 1.0
        sel[32 + e, e, :] = 1.0
    c["c_sel"] = sel
    k = np.arange(128)[:, None, None]
    kb = np.arange(4)[None, :, None]
    q = np.arange(512)[None, None, :]
    c["c_tri4"] = ((128 * kb + k) <= q).astype(f)
    return c


def _core_tables(j):
    f = np.float32
    sbs = [j, 7 - j, 8 + j, 15 - j]
    mcoef = np.zeros((32,), f)
    for s in range(4):
        for u in range(4):
            t = 4 * s + u
            a, b = (1.0, 0.0) if t < sbs[s] else ((0.0, 1.0) if t == sbs[s] else (0.0, 0.0))
            mcoef[(s * 4 + u) * 2] = a
            mcoef[(s * 4 + u) * 2 + 1] = b
    ab = np.zeros((128, NAB), f)
    p = np.arange(128, dtype=np.float64)
    for h in range(8):
        W = WH[h]
        for s in range(4):
            for r in range(512 // W):
                ref = 512 * sbs[s] + r * W + W / 2
                base = ABOFF[(h, s, r)]
                for kbg in range(4 * KMAX[s]):
                    v = SLOPES[h] * (128 * kbg + p - ref)
                    ab[:, base + kbg] = np.minimum(v, SLOPES[h] * W / 2)
    return sbs, np.ascontiguousarray(np.broadcast_to(mcoef[None], (128, 32))), ab


def kernel(**inputs):
    inp = {k: np.asarray(v) for k, v in inputs.items()}
    x = inp["x"]
    if "nc" not in _NC_CACHE:
        _NC_CACHE["nc"] = build_nc()
    nc = _NC_CACHE["nc"]
    consts = _host_consts(inp)
    shared = {
        "w_in": np.ascontiguousarray(inp["w_in"][0]), "w_gate": np.ascontiguousarray(inp["w_gate"][0]),
        "w_up_a": np.ascontiguousarray(inp["w_up_a"][0]), "w_up_b": np.ascontiguousarray(inp["w_up_b"][0]),
        "w_out": np.ascontiguousarray(inp["w_out"][0]),
    }
    w13 = np.concatenate([inp["w1"][0].reshape(NE, 8, 128, 256), inp["w3"][0].reshape(NE, 8, 128, 256)], axis=3)
    w13 = w13.transpose(0, 2, 1, 3).reshape(NE, 128, 4096)
    w2p = inp["w2"][0].reshape(NE, 2, 128, 1024).transpose(0, 2, 1, 3).reshape(NE, 128, 2048)
    shared["ewp"] = np.ascontiguousarray(np.concatenate([w13, w2p], axis=2))
    shared.update(consts)
    in_maps = []
    own = []
    for c in range(8):
        b, j = c // 4, c % 4
        sbs, mcoef, ab = _core_tables(j)
        xo = np.concatenate([x[b, sb * 512:(sb + 1) * 512] for sb in sbs], axis=0)
        m = dict(shared)
        m["xf"] = np.ascontiguousarray(x[b])
        m["xo"] = np.ascontiguousarray(xo)
        m["c_mcoef"] = mcoef
        m["c_abias"] = ab
        in_maps.append(m)
        own.append((b, sbs))
    res = run_bass_kernel_spmd(nc, in_maps, core_ids=list(range(8)))
    outp = np.empty((2, SEQ, D), np.float32)
    for c in range(8):
        b, sbs = own[c]
        o = np.asarray(res.results[c]["out"])
        for i, sb in enumerate(sbs):
            outp[b, sb * 512:(sb + 1) * 512] = o[i * 512:(i + 1) * 512]
    return outp
```

```python
import os
import numpy as np
import concourse.bass as bass
import concourse.mybir as mybir
from concourse.bass_utils import run_bass_kernel_spmd

F32 = mybir.dt.float32
BF16 = mybir.dt.bfloat16
AF = mybir.ActivationFunctionType
ALU = mybir.AluOpType
AX = mybir.AxisListType

D = 1024
SEQ = 8192
NSB = 16
KMAX = [4, 8, 12, 16]
EPS = 1e-6
NE = 32
WH = [256, 512, 512, 512, 512, 512, 512, 512]
SLOPES = [2.0 ** (-(h + 1)) for h in range(8)]


def abias_layout():
    off = {}
    n = 0
    for h in range(8):
        nsub = 512 // WH[h]
        for s in range(4):
            for r in range(nsub):
                off[(h, s, r)] = n
                n += 4 * KMAX[s]
    return off, n


ABOFF, NAB = abias_layout()


class Buf:
    def __init__(self, name, dram=False):
        self.name = name
        self.dram = dram
        self.w = {}
        self.r = {}
        self.dsem = None
        self.dcnt = 0


class Sched:
    CE = ("pe", "act", "dve", "pool")

    def __init__(self, nc):
        self.nc = nc
        self.E = {"pe": nc.tensor, "act": nc.scalar, "dve": nc.vector, "pool": nc.gpsimd, "sp": nc.sync}
        self.sem = {k: nc.alloc_semaphore(name="cs_" + k) for k in self.CE}
        self.n = {k: 0 for k in self.CE}
        self.last = {k: None for k in self.CE}
        self.sig = {k: [] for k in self.CE}
        self.seen = {k: {} for k in self.E}
        self.dsems = []

    def _value_for(self, F, i):
        sl = self.sig[F]
        lo, hi = 0, len(sl)
        while lo < hi:
            mid = (lo + hi) // 2
            if sl[mid] >= i:
                hi = mid
            else:
                lo = mid + 1
        if lo < len(sl):
            return lo + 1
        self._signal_last(F)
        return len(sl)

    def _signal_last(self, F):
        if self.sig[F] and self.sig[F][-1] == self.n[F]:
            return
        self.last[F].then_inc(self.sem[F], 1)
        self.sig[F].append(self.n[F])

    def _wait(self, E, tok):
        if tok is None:
            return
        if tok[0] == "dma":
            _, sem, val, key = tok
            if self.seen[E].get(key, 0) >= val:
                return
            self.E[E].wait_ge(sem, val)
            self.seen[E][key] = val
            return
        F, i = tok
        if F == E and E == "pe":
            return
        val = self._value_for(F, i)
        if self.seen[E].get(F, 0) >= val:
            return
        self.E[E].wait_ge(self.sem[F], val)
        self.seen[E][F] = val

    def _deps(self, E, R, W, join=False):
        for b in R:
            for t in list(b.w.values()):
                self._wait(E, t)
        for b in W:
            if not join:
                for t in list(b.w.values()):
                    self._wait(E, t)
            for t in list(b.r.values()):
                self._wait(E, t)

    def op(self, E, fn, R=(), W=(), sig=False):
        self._deps(E, R, W)
        ins = fn()
        self.n[E] += 1
        self.last[E] = ins
        tok = (E, self.n[E])
        for b in R:
            b.r[E] = tok
        for b in W:
            b.w = {E: tok}
            b.r = {}
        if sig:
            self._signal_last(E)
        return ins

    def dma(self, q, out, in_, R=(), W=(), join=False):
        self._deps(q, R, W, join=join)
        sb = W[0] if W else R[0]
        if W and W[0].dram and R:
            sb = R[0]
        if q == "pool":
            self.nsw = getattr(self, "nsw", 0) + 1
            sb = Buf("sw%d_%s" % (self.nsw, sb.name))
            assert not join
        if sb.dsem is None:
            sb.dsem = self.nc.alloc_semaphore(name="ds_" + sb.name)
            self.dsems.append(sb)
        sb.dcnt += 16
        self.E[q].dma_start(out=out, in_=in_).then_inc(sb.dsem, 16)
        tok = ("dma", sb.dsem, sb.dcnt, sb.name)
        for b in R:
            b.r[("dma", sb.name)] = tok
        for b in W:
            if join:
                b.w[("dma", sb.name)] = tok
            else:
                b.w = {("dma", sb.name): tok}
                b.r = {}
        return tok

    def barrier(self):
        for F in self.CE:
            if self.last[F] is not None:
                self._signal_last(F)
        for E in self.E:
            for F in self.CE:
                if self.last[F] is None or F == E:
                    continue
                val = len(self.sig[F])
                if self.seen[E].get(F, 0) < val:
                    self.E[E].wait_ge(self.sem[F], val)
                    self.seen[E][F] = val
            for sb in self.dsems:
                if self.seen[E].get(sb.name, 0) < sb.dcnt:
                    self.E[E].wait_ge(sb.dsem, sb.dcnt)
                    self.seen[E][sb.name] = sb.dcnt

    def finish(self, toks):
        for t in toks:
            self._wait("sp", t)


def build_nc(upto=None, debug=False):
    nc = bass.Bass("TRN2", target_bir_lowering=False)
    S = Sched(nc)

    def din(name, shape, dt=F32):
        return nc.dram_tensor(name, list(shape), dt, kind="ExternalInput").ap()

    xf = din("xf", [SEQ, D])
    xo = din("xo", [2048, D])
    w_in = din("w_in", [D, 5120])
    w_gate = din("w_gate", [D, 2048])
    w_up_a = din("w_up_a", [D, D])
    w_up_b = din("w_up_b", [D, D])
    w_out = din("w_out", [D, D])
    ewp = din("ewp", [NE, 128, 6144])
    c_ident = din("c_ident", [128, 128])
    c_g1 = din("c_g1", [128, 8])
    c_g2 = din("c_g2", [128, 8])
    c_gqk = din("c_gqk", [128, 2])
    c_gv = din("c_gv", [128, 1024])
    c_wsT = din("c_wsT", [128, 8, 128])
    c_triu = din("c_triu", [128, 128])
    c_bs = din("c_bs", [128, 8, 128])
    c_bgate = din("c_bgate", [128, 16])
    c_gsub = din("c_gsub", [128, 1])
    c_lam = din("c_lam", [128, 4, 64])
    c_br = din("c_br", [128, 36])
    c_wr = din("c_wr", [128, 8, 36])
    c_sel = din("c_sel", [64, NE, 128])
    c_tri4 = din("c_tri4", [128, 4, 512])
    c_mcoef = din("c_mcoef", [128, 32])
    c_abias = din("c_abias", [128, NAB])
    out = nc.dram_tensor("out", [2048, D], F32, kind="ExternalOutput").ap()

    def dscr(name, shape, dt=BF16):
        return nc.dram_tensor(name, list(shape), dt, kind=("ExternalOutput" if debug else "Internal")).ap()

    hTs = dscr("hTs", [4, 128, 8 * 512])
    QTs = dscr("QTs", [4, 128, 8 * 512])
    yaTs = dscr("yaTs", [4, 128, 8 * 512])
    ATs = dscr("ATs", [4, 128, 8 * 512])
    mTs = dscr("mTs", [4, 128, 8 * 512])
    KTs = dscr("KTs", [NSB, 128, 8 * 512])
    Vs = dscr("Vs", [NSB, 128, 8 * 4 * 129])

    def sb(name, shape, dt, off):
        return nc.alloc_sbuf_tensor_at(name, list(shape), dt, offset=off)

    BASE = 16512
    R_A = BASE
    R_B = R_A + 65536
    R_W = R_B + 32768
    R_C = R_W + 49152
    R_S = R_C + 32768

    co = [R_C]

    def calloc(name, shape, dt):
        nbytes = int(np.prod(shape[1:])) * (4 if dt == F32 else 2)
        nbytes = (nbytes + 31) // 32 * 32
        t = sb(name, shape, dt, co[0])
        co[0] += nbytes
        assert co[0] <= R_S, name
        return t

    ident_f = calloc("ident_f", [128, 128], F32)
    ident_b = calloc("ident_b", [128, 128], BF16)
    g1c = calloc("g1c", [128, 8], F32)
    g2c = calloc("g2c", [128, 8], F32)
    gqk = calloc("gqk", [128, 2], F32)
    gv_bc = calloc("gv_bc", [128, 1024], F32)
    wsT_m = calloc("wsT_m", [128, 8, 128], BF16)
    bs_t = calloc("bs_t", [128, 8, 128], F32)
    bgate = calloc("bgate", [128, 16], F32)
    gsub8 = calloc("gsub8", [128, 1], F32)
    ones_b = calloc("ones_b", [128, 128], BF16)
    br_t = calloc("br_t", [128, 36], F32)
    wr_t = calloc("wr_t", [128, 8, 36], F32)
    sel_t = calloc("sel_t", [64, NE, 128], BF16)
    mcoef = calloc("mcoef", [128, 32], F32)
    zeros_b = calloc("zeros_b", [128, 640], BF16)
    epsT = calloc("epsT", [128, 1], F32)
    neglam = calloc("neglam", [128, 1], F32)
    cT = calloc("cT", [64, 2048], BF16)
    CONST = Buf("const")

    PA = nc.alloc_psum_tensor("PA", [128, 1024], F32)
    PB = nc.alloc_psum_tensor("PB", [128, 1024], F32)
    PC = nc.alloc_psum_tensor("PC", [128, 1024], F32)
    PD = nc.alloc_psum_tensor("PD", [128, 1024], F32)
    bPA0, bPA1, bPB0, bPB1, bPC0, bPC1, bPD, bPD1 = [Buf("ps%d" % i) for i in range(8)]

    def MM(out_, lhsT, rhs, start, stop, R, W, sig=False, **kw):
        return S.op("pe", lambda: nc.tensor.matmul(out_, lhsT=lhsT, rhs=rhs, start=start, stop=stop, **kw), R, W, sig)

    def TR(out_, in_, ident, R, W, sig=False):
        return S.op("pe", lambda: nc.tensor.transpose(out_, in_, ident), R, W, sig)

    def ACT(out_, in_, func, R, W, bias=None, scale=None, sig=False):
        kw = {}
        if bias is not None:
            kw["bias"] = bias
        if scale is not None:
            kw["scale"] = scale
        return S.op("act", lambda: nc.scalar.activation(out=out_, in_=in_, func=func, **kw), R, W, sig)

    def V(name, R, W, sig=False, eng="dve", **kw):
        e = nc.vector if eng == "dve" else nc.gpsimd
        return S.op(eng, lambda: getattr(e, name)(**kw), R, W, sig)

    lam_t = sb("lam_t", [128, 4, 64], F32, R_A)
    lam_p = sb("lam_p", [128, 2, 64], F32, R_A + 1024)
    lam_s = sb("lam_s", [128, 2], F32, R_A + 1536)
    lam_e = sb("lam_e", [128, 2], F32, R_A + 1568)
    wsT_f = sb("wsT_f", [128, 8, 128], F32, R_A + 2048)
    triu_f = sb("triu_f", [128, 128], F32, R_A + 6144)
    gsub_f = sb("gsub_f", [128, 1], F32, R_A + 6656)
    SETUP = Buf("setup")
    for dst, src in [(ident_f, c_ident), (g1c, c_g1), (g2c, c_g2), (gqk, c_gqk), (gv_bc, c_gv), (bs_t, c_bs),
                     (bgate, c_bgate), (br_t, c_br), (wr_t, c_wr), (mcoef, c_mcoef)]:
        S.dma("sp", dst[:], src, W=[CONST], join=True)
    for dst, src in [(lam_t, c_lam), (wsT_f, c_wsT), (triu_f, c_triu), (gsub_f, c_gsub)]:
        S.dma("sp", dst[:], src, W=[SETUP], join=True)
    CSEL = Buf("csel")
    CIDB = Buf("cidb")
    S.dma("pool", sel_t[:], c_sel, W=[CSEL])
    S.dma("pool", ident_b[:], c_ident, W=[CIDB])
    C2 = Buf("const2")
    V("memset", [], [C2], ap=zeros_b[:], constant=0.0)
    V("memset", [], [C2], ap=epsT[:], constant=EPS)
    V("memset", [], [C2], ap=ones_b[:], constant=1.0)
    V("tensor_tensor", [SETUP], [C2], out=wsT_m[:], in0=wsT_f[:], in1=triu_f[:].unsqueeze(1).to_broadcast([128, 8, 128]), op=ALU.mult)
    V("tensor_scalar", [SETUP], [C2], out=gsub8[:], in0=gsub_f[:], scalar1=0.8, scalar2=None, op0=ALU.mult)
    V("tensor_tensor", [SETUP], [C2], out=lam_p[:, 0, :], in0=lam_t[:, 0, :], in1=lam_t[:, 1, :], op=ALU.mult)
    V("tensor_tensor", [SETUP], [C2], out=lam_p[:, 1, :], in0=lam_t[:, 2, :], in1=lam_t[:, 3, :], op=ALU.mult)
    V("tensor_reduce", [C2], [C2], out=lam_s[:], in_=lam_p[:], axis=AX.X, op=ALU.add)
    ACT(lam_e[:], lam_s[:], AF.Exp, [C2], [C2])
    V("tensor_tensor", [C2], [C2], out=neglam[:], in0=lam_e[:, 1:2], in1=lam_e[:, 0:1], op=ALU.subtract)
    V("tensor_scalar", [C2], [C2], out=neglam[:], in0=neglam[:], scalar1=-0.2, scalar2=None, op0=ALU.add)
    CR = [CONST, C2, CSEL, CIDB]
    S.barrier()
    if upto == "setup":
        return nc

    WS = [sb("wslot%d" % i, [128, 8, 1024], BF16, R_W + i * 16384) for i in range(3)]
    bWS = [Buf("wslot%d" % i) for i in range(3)]

    def load_w(slot, src_cols):
        S.dma("pool", WS[slot][:], src_cols.rearrange("(c p) n -> p c n", p=128), W=[bWS[slot]])

    XT = [sb("xt%d" % i, [128, 4, 1024], F32, R_A + i * 16384) for i in range(2)]
    bXT = [Buf("xt%d" % i) for i in range(2)]
    XS = sb("xs4", [128, 4, 1024], F32, R_A + 32768)
    bXS = Buf("xs4")
    SQ = sb("sq", [128, 1024], F32, R_A + 49152)
    bSQ = Buf("sq")
    HT = [sb("hT%d" % i, [128, 8, 512], BF16, R_S + i * 8192) for i in range(2)]
    bHT = [Buf("hT%d" % i) for i in range(2)]
    st_ss = sb("st_ss", [128, 64], F32, R_A + 53248)
    st_sq = sb("st_sq", [128, 64], F32, R_A + 53504)
    st_r = sb("st_r", [128, 64], F32, R_A + 53760)
    bST = Buf("stats")

    SQs = [SQ, sb("sq2", [128, 1024], F32, R_A + 57344)]
    bSQs = [bSQ, Buf("sq2")]
    bSTs = [Buf("st%d" % i) for i in range(4)]
    st_cnt = [0]

    def rms_head(src_ap, ngroups, glen, R):
        k = st_cnt[0]
        st_cnt[0] += 1
        sq, bsq = SQs[k % 2], bSQs[k % 2]
        c0 = 16 * (k % 4)
        bst = bSTs[k % 4]
        ACT(sq[:, 0:ngroups * glen], src_ap, AF.Square, R, [bsq])
        V("tensor_reduce", [bsq], [bst], out=st_ss[:, c0:c0 + ngroups],
          in_=sq[:, 0:ngroups * glen].rearrange("p (g d) -> p g d", d=glen), axis=AX.X, op=ALU.add)
        return (c0, ngroups, glen, bst)

    def rms_tail(ctx):
        c0, ngroups, glen, bst = ctx
        ACT(st_sq[:, c0:c0 + ngroups], st_ss[:, c0:c0 + ngroups], AF.Sqrt, [bst] + CR, [bst],
            bias=epsT[:, 0:1], scale=1.0 / glen)
        V("reciprocal", [bst], [bst], out=st_r[:, c0:c0 + ngroups], in_=st_sq[:, c0:c0 + ngroups])
        return st_r[:, c0:c0 + ngroups], bst

    def rms_stats(src_ap, ngroups, glen, R):
        return rms_tail(rms_head(src_ap, ngroups, glen, R))

    load_w(0, w_in[:, 3072:4096])
    load_w(1, w_in[:, 4096:5120])
    KTsb = [sb("ktsb%d" % i, [128, 8, 512], BF16, R_B + i * 8192) for i in range(2)]
    bKTsb = [Buf("ktsb%d" % i) for i in range(2)]
    Vsb = [sb("vsb%d" % i, [128, 8, 4, 129], BF16, R_B + 16384 + i * 8256) for i in range(1)]
    bVsb = [Buf("vsb0")]
    V("memset", [], [bVsb[0]], ap=Vsb[0][:], constant=1.0)
    KNT = sb("knt", [128, 4, 1024], F32, R_W + 32768)
    bKNT = Buf("knt")
    bKTs = [Buf("KVscr", dram=True)] * NSB
    bVs = bKTs

    def load_x(i, src_rows):
        S.dma("sp", XT[i % 2][:], src_rows.rearrange("(b p) d -> p b d", p=128), W=[bXT[i % 2]])

    def qk_norm_block(ps, bps, dst_ap, R_extra, W):
        ctx = rms_head(ps, 8, 64, [bps])

        def tail():
            r_, bst_ = rms_tail(ctx)
            V("tensor_tensor", [bps, bst_], W, out=dst_ap.rearrange("p (g d) -> p g d", d=64),
              in0=ps.rearrange("p (g d) -> p g d", d=64),
              in1=r_.unsqueeze(2).to_broadcast([128, 8, 64]), op=ALU.mult)
        return tail

    def norm_part(xt, bxt):
        prev = None
        for tb in range(4):
            ctx = rms_head(xt[:, tb, :], 1, 1024, [bxt])
            if prev is not None:
                r_, bst_ = rms_tail(prev[1])
                V("tensor_scalar", [bxt, bst_], [bXS], out=XS[:, prev[0], :], in0=xt[:, prev[0], :], scalar1=r_[:, 0:1],
                  scalar2=None, op0=ALU.mult)
            prev = (tb, ctx)
        r_, bst_ = rms_tail(prev[1])
        V("tensor_scalar", [bxt, bst_], [bXS], out=XS[:, prev[0], :], in0=xt[:, prev[0], :], scalar1=r_[:, 0:1],
          scalar2=None, op0=ALU.mult)

    def tx_part(hT, bhT, gcol):
        for c in range(8):
            pt, bpt = (PA[:, 0:512], bPA0) if c % 2 == 0 else (PA[:, 512:1024], bPA1)
            for tb in range(4):
                TR(pt[:, tb * 128:(tb + 1) * 128], XS[:, tb, c * 128:(c + 1) * 128], ident_f[:], [bXS] + CR, [bpt], sig=(tb == 3))
            V("tensor_scalar", [bpt] + CR, [bhT], out=hT[:, c, :], in0=pt, scalar1=gcol[:, c:c + 1], scalar2=None, op0=ALU.mult)

    KB4 = [(PB[:, 0:512], bPB0), (PB[:, 512:1024], bPB1), (PD[:, 0:512], bPD), (PD[:, 512:1024], bPD1)]

    pend = [None]

    def kv_kproj(sbi):
        hT, bhT = HT[sbi % 2], bHT[sbi % 2]
        for tb in range(4):
            for half in range(2):
                ps, bps = KB4[(tb * 2 + half) % 4]
                for c in range(8):
                    MM(ps, hT[:, c, tb * 128:(tb + 1) * 128], WS[0][:, c, half * 512:(half + 1) * 512], c == 0, c == 7,
                       [bhT, bWS[0]], [bps], sig=(c == 7))
                t_ = qk_norm_block(ps, bps, KNT[:, tb, half * 512:(half + 1) * 512], [], [bKNT])
                if pend[0] is not None:
                    pend[0]()
                pend[0] = t_
        pend[0]()
        pend[0] = None

    def kv_vproj(sbi):
        hT, bhT = HT[sbi % 2], bHT[sbi % 2]
        vs, bvs = Vsb[0], bVsb[0]
        for tb in range(4):
            for half in range(2):
                ps, bps = (PC[:, 0:512], bPC0) if half == 0 else (PC[:, 512:1024], bPC1)
                for c in range(8):
                    MM(ps, hT[:, c, tb * 128:(tb + 1) * 128], WS[1][:, c, half * 512:(half + 1) * 512], c == 0, c == 7,
                       [bhT, bWS[1]], [bps], sig=(c == 7))
                ACT(vs[:, half * 4:(half + 1) * 4, tb, 0:128], ps.rearrange("p (h d) -> p h d", d=128), AF.Copy, [bps], [bvs])
        S.dma("sp", Vs[sbi], vs[:].rearrange("p h b d -> p (h b d)"), R=[bvs], W=[bVs[sbi]], join=True)

    def kv_tk(sbi):
        kts, bkts = KTsb[sbi % 2], bKTsb[sbi % 2]
        for h in range(8):
            pt, bpt = (PA[:, 0:512], bPA0) if h % 2 == 0 else (PA[:, 512:1024], bPA1)
            for tb in range(4):
                TR(pt[:, tb * 128:(tb + 1) * 128], KNT[:, tb, h * 128:(h + 1) * 128], ident_f[:], [bKNT] + CR, [bpt], sig=(tb == 3))
            ACT(kts[:, h, :], pt, AF.Copy, [bpt] + CR, [bkts], scale=gqk[:, 1:2])
        S.dma("sp", KTs[sbi], kts[:].rearrange("p h t -> p (h t)"), R=[bkts], W=[bKTs[sbi]], join=True)

    load_x(0, xf[0:512, :])
    load_x(1, xf[512:1024, :])
    norm_part(XT[0], bXT[0])
    tx_part(HT[0], bHT[0], g1c)
    for sbi in range(NSB):
        if sbi + 2 < NSB:
            load_x(sbi + 2, xf[(sbi + 2) * 512:(sbi + 3) * 512, :])
        if sbi + 1 < NSB:
            norm_part(XT[(sbi + 1) % 2], bXT[(sbi + 1) % 2])
        kv_kproj(sbi)
        kv_vproj(sbi)
        if sbi + 1 < NSB:
            tx_part(HT[(sbi + 1) % 2], bHT[(sbi + 1) % 2], g1c)
        kv_tk(sbi)
    S.barrier()
    if upto == "kv":
        return nc

    bhTs = [Buf("hTs", dram=True)] * 4
    bQTs = [Buf("QTs", dram=True)] * 4
    byaTs = [Buf("yaTs", dram=True)] * 4
    bATs = [Buf("ATs", dram=True)] * 4
    bmTs = [Buf("mTs", dram=True)] * 4
    load_w(0, w_in[:, 2048:3072])
    load_w(1, w_in[:, 0:1024])
    load_w(2, w_in[:, 1024:2048])
    QTsb = KTsb
    bQTsb = bKTsb
    def a1_qproj(s):
        hT, bhT = HT[s % 2], bHT[s % 2]
        for tb in range(4):
            for half in range(2):
                ps, bps = KB4[(tb * 2 + half) % 4]
                for c in range(8):
                    MM(ps, hT[:, c, tb * 128:(tb + 1) * 128], WS[0][:, c, half * 512:(half + 1) * 512], c == 0, c == 7,
                       [bhT, bWS[0]], [bps], sig=(c == 7))
                t_ = qk_norm_block(ps, bps, KNQ[:, tb, half * 512:(half + 1) * 512], [], [bKNQ])
                if pend[0] is not None:
                    pend[0]()
                pend[0] = t_
        pend[0]()
        pend[0] = None

    def a1_tq(s):
        qts, bqts = QTsb[s % 2], bQTsb[s % 2]
        for h in range(8):
            pt, bpt = (PC[:, 0:512], bPC0) if h % 2 == 0 else (PC[:, 512:1024], bPC1)
            for tb in range(4):
                TR(pt[:, tb * 128:(tb + 1) * 128], KNQ[:, tb, h * 128:(h + 1) * 128], ident_f[:], [bKNQ] + CR, [bpt], sig=(tb == 3))
            ACT(qts[:, h, :], pt, AF.Copy, [bpt] + CR, [bqts], scale=gqk[:, 0:1])
        S.dma("sp", QTs[s], qts[:].rearrange("p h t -> p (h t)"), R=[bqts], W=[bQTs[s]], join=True)

    KNQ = sb("knq", [128, 4, 1024], F32, R_B + 16384)
    bKNQ = Buf("knq")
    load_x(0, xo[0:512, :])
    load_x(1, xo[512:1024, :])
    norm_part(XT[0], bXT[0])
    tx_part(HT[0], bHT[0], g1c)
    for s in range(4):
        hT, bhT = HT[s % 2], bHT[s % 2]
        S.dma("sp", hTs[s], hT[:].rearrange("p c t -> p (c t)"), R=[bhT], W=[bhTs[s]], join=True)
        if s + 2 < 4:
            load_x(s + 2, xo[(s + 2) * 512:(s + 3) * 512, :])
        if s + 1 < 4:
            norm_part(XT[(s + 1) % 2], bXT[(s + 1) % 2])
        a1_qproj(s)
        if s + 1 < 4:
            tx_part(HT[(s + 1) % 2], bHT[(s + 1) % 2], g1c)
        a1_tq(s)
    S.barrier()
    if upto == "a1":
        return nc
    load_w(0, w_up_a)
    WS3 = sb("wslot3", [128, 8, 1024], BF16, R_B + 16384)
    bWS3 = Buf("wslot3")
    S.dma("pool", WS3[:], w_gate[:, 0:1024].rearrange("(c p) n -> p c n", p=128), W=[bWS3])
    AIN = [sb("ain%d" % i, [128, 8, 512], BF16, R_A + i * 8192) for i in range(4)]
    bAIN = [Buf("ain%d" % i) for i in range(4)]
    VN = sb("vn", [128, 4, 1024], BF16, R_A + 32768)
    bVN = Buf("vn")
    UT = sb("uT", [128, 8, 512], BF16, R_A + 40960)
    bUT = Buf("uT")
    TMP = sb("tmpf", [128, 512], F32, R_A + 55296)
    bTMP = Buf("tmpf")
    TMP2 = sb("tmpf2", [128, 512], F32, R_A + 57344 + 4096)
    bTMP2 = Buf("tmpf2")
    AOUT = [sb("aout%d" % i, [128, 8, 512], BF16, R_B + i * 8192) for i in range(2)]
    bAOUT = [Buf("aout%d" % i) for i in range(2)]

    def load_act(i, src, bsrc):
        S.dma("sp", AIN[i][:].rearrange("p c t -> p (c t)"), src, R=[bsrc], W=[bAIN[i]])

    load_act(0, hTs[0], bhTs[0])
    for s in range(4):
        if s + 1 < 4:
            load_act((s + 1) % 2, hTs[s + 1], bhTs[s + 1])
        hT, bhT = AIN[s % 2], bAIN[s % 2]
        ya, bya = AOUT[s % 2], bAOUT[s % 2]
        for m in range(8):
            ps, bps = (PA[:, 0:512], bPA0) if m % 2 == 0 else (PA[:, 512:1024], bPA1)
            for c in range(8):
                MM(ps, WS[1][:, c, m * 128:(m + 1) * 128], hT[:, c, :], c == 0, c == 7, [bhT, bWS[1]], [bps], sig=(c == 7))
            ACT(UT[:, m, :], ps, AF.Copy, [bps], [bUT])
        for tb in range(4):
            for half in range(2):
                ps, bps = (PB[:, 0:512], bPB0) if half == 0 else (PB[:, 512:1024], bPB1)
                for c in range(8):
                    MM(ps, hT[:, c, tb * 128:(tb + 1) * 128], WS[2][:, c, half * 512:(half + 1) * 512], c == 0, c == 7,
                       [bhT, bWS[2]], [bps], sig=(c == 7))
                r_, bst_ = rms_stats(ps, 4, 128, [bps])
                V("tensor_tensor", [bps, bst_], [bTMP], out=TMP[:].rearrange("p (g d) -> p g d", d=128),
                  in0=ps.rearrange("p (g d) -> p g d", d=128),
                  in1=r_.unsqueeze(2).to_broadcast([128, 4, 128]), op=ALU.mult)
                V("tensor_tensor", [bTMP] + CR, [bVN], out=VN[:, tb, half * 512:(half + 1) * 512], in0=TMP[:],
                  in1=gv_bc[:, half * 512:(half + 1) * 512], op=ALU.mult)
        for g in range(8):
            ps, bps = (PC[:, 0:512], bPC0) if g % 2 == 0 else (PC[:, 512:1024], bPC1)
            for tb in range(4):
                MM(ps[:, tb * 128:(tb + 1) * 128], VN[:, tb, g * 128:(g + 1) * 128], wsT_m[:, g, :], True, True,
                   [bVN] + CR, [bps], sig=(tb == 3))
            V("tensor_tensor", [bps] + CR, [bTMP2], out=TMP2[:].rearrange("p (b t) -> p b t", t=128),
              in0=ps.rearrange("p (b t) -> p b t", t=128),
              in1=bs_t[:, g, :].unsqueeze(1).to_broadcast([128, 4, 128]), op=ALU.add)
            V("tensor_tensor", [bTMP2, bUT], [bya], out=ya[:, g, :], in0=TMP2[:], in1=UT[:, g, :], op=ALU.mult)
        S.dma("sp", yaTs[s], ya[:].rearrange("p c t -> p (c t)"), R=[bya], W=[byaTs[s]], join=True)
    S.barrier()
    if upto == "a2":
        return nc
    SG = sb("sgf", [128, 512], F32, R_A + 49152)
    bSG = Buf("sgf")
    load_act(0, hTs[0], bhTs[0])
    load_act(2, yaTs[0], byaTs[0])
    for s in range(4):
        if s + 1 < 4:
            load_act((s + 1) % 2, hTs[s + 1], bhTs[s + 1])
            load_act(2 + (s + 1) % 2, yaTs[s + 1], byaTs[s + 1])
        hT, bhT = AIN[s % 2], bAIN[s % 2]
        ya, bya = AIN[2 + s % 2], bAIN[2 + s % 2]
        at, bat = AOUT[s % 2], bAOUT[s % 2]
        for m in range(8):
            psu, bpsu = (PA[:, 0:512], bPA0) if m % 2 == 0 else (PA[:, 512:1024], bPA1)
            psg, bpsg = (PB[:, 0:512], bPB0) if m % 2 == 0 else (PB[:, 512:1024], bPB1)
            for c in range(8):
                MM(psg, WS3[:, c, m * 128:(m + 1) * 128], hT[:, c, :], c == 0, c == 7, [bhT, bWS3], [bpsg], sig=(c == 7))
            for c in range(8):
                MM(psu, WS[0][:, c, m * 128:(m + 1) * 128], ya[:, c, :], c == 0, c == 7, [bya, bWS[0]], [bpsu], sig=(c == 7))
            ACT(SG[:], psg, AF.Sigmoid, [bpsg] + CR, [bSG], bias=bgate[:, m:m + 1])
            V("tensor_tensor", [bSG, bpsu], [bat], out=at[:, m, :], in0=SG[:], in1=psu, op=ALU.mult)
        S.dma("sp", ATs[s], at[:].rearrange("p c t -> p (c t)"), R=[bat], W=[bATs[s]], join=True)
    S.barrier()
    if upto == "a3":
        return nc

    YBR = sb("ybraw", [128, 8, 2048], BF16, R_B)
    bYBR = [Buf("ybraw%d" % i) for i in range(4)]
    MK = sb("mk", [128, 16, 512], BF16, R_W)
    bMK = Buf("mk")
    TRI = sb("tri4", [128, 4, 512], BF16, R_W + 16384)
    ABI = sb("abias", [128, NAB], F32, R_W + 20480)
    ATTC = Buf("attc")
    ATTC2 = Buf("attc2")
    S.dma("pool", TRI[:], c_tri4, W=[ATTC2])
    S.dma("sp", ABI[:], c_abias, W=[ATTC])
    load_w(2, w_out)
    WS4 = sb("wslot4", [128, 8, 1024], BF16, R_S + 8192)
    bWS4 = Buf("wslot4")
    S.dma("pool", WS4[:], w_gate[:, 1024:2048].rearrange("(c p) n -> p c n", p=128), W=[bWS4])
    NPT = 3
    PT = [sb("pT%d" % i, [128, 2, 512], BF16, R_A + 32768 + i * 2048) for i in range(NPT)]
    bPT = [Buf("pT%d" % i) for i in range(NPT)]
    NKB = 4
    KTb = [sb("ktb%d" % i, [128, 512], BF16, R_A + 4096 + i * 1024) for i in range(NKB)]
    bKTb = [Buf("ktb%d" % i) for i in range(NKB)]
    VB = [sb("vb%d" % i, [128, 4, 129], BF16, R_A + 8192 + i * 1056) for i in range(NKB)]
    bVB = [Buf("vb%d" % i) for i in range(NKB)]
    QKFULL = bool(os.environ.get("KDBG_QKFULL", "0") == "1")
    QB = [sb("qb%d" % i, [128, 2, 512], BF16, R_A + i * 2048) for i in range(2)]
    bQB = [Buf("qb%d" % i) for i in range(2)]
    for i in range(2):
        V("memset", [], [bQB[i]], ap=QB[i][:], constant=0.0)

    def load_q(i, s_, h_):
        S.dma("sp", QB[i][0:64, 0, :], QTs[s_][0:64, h_ * 512:(h_ + 1) * 512], R=[bQTs[s_]], W=[bQB[i]])
        S.dma("sp", QB[i][64:128, 1, :], QTs[s_][64:128, h_ * 512:(h_ + 1) * 512], R=[bQTs[s_]], W=[bQB[i]], join=True)
    RZ = sb("rz", [128, 2, 512], F32, R_A + 16384)
    FT = sb("ft", [128, 2, 512], F32, R_A + 20480)
    bRZ = Buf("rz")
    bFT = Buf("ft")
    ZC = sb("zc", [128, 2, 512], F32, R_A + 24576)
    OC = sb("oc", [128, 2, 512], F32, R_A + 28672)
    bZC = Buf("zc")
    bOC = Buf("oc")
    OACC = [(PC[:, 0:512], bPC0), (PC[:, 512:1024], bPC1)]
    ZACC = [(PD[:, 0:512], bPD), (PD[:, 512:1024], bPD1)]

    loads = [(s, h, t) for s in range(4) for h in range(8) for t in range(KMAX[s])]

    def issue_load(i):
        s, h, t = loads[i]
        S.dma("sp", KTb[i % NKB][:], KTs[t][:, h * 512:(h + 1) * 512], R=[bKTs[t]], W=[bKTb[i % NKB]])
        S.dma("sp", VB[i % NKB][:].rearrange("p b d -> p (b d)"), Vs[t][:, h * 516:(h + 1) * 516], R=[bVs[t]], W=[bVB[i % NKB]])

    li = 0
    issue_load(0)
    issue_load(1)
    qi = 0
    load_q(0, 0, 0)
    for s in range(4):
        for u in range(4):
            for kb in range(4):
                V("tensor_scalar", [ATTC2] + CR, [bMK], eng="pool", out=MK[:, u * 4 + kb, :], in0=TRI[:, kb, :],
                  scalar1=mcoef[:, (s * 4 + u) * 2 + 1:(s * 4 + u) * 2 + 2], scalar2=mcoef[:, (s * 4 + u) * 2:(s * 4 + u) * 2 + 1],
                  op0=ALU.mult, op1=ALU.add)
        for h in range(8):
            qb, bqb = QB[qi % 2], bQB[qi % 2]
            nxt = qi + 1
            if nxt < 32:
                s2, h2 = nxt // 8, nxt % 8
                load_q(nxt % 2, s2, h2)
            qi += 1
            items = [(t, kb) for t in range(KMAX[s]) for kb in range(4)]
            W_ = WH[h]
            nsub = 512 // W_
            nit = len(items)

            def QK(i, li_t):
                t, kb = items[i]
                kt, bkt = KTb[li_t % NKB], bKTb[li_t % NKB]
                st, bst0, bst1 = (PA, bPA0, bPA1) if i % 2 == 0 else (PB, bPB0, bPB1)
                if QKFULL:
                    MM(st[:, 0:512], kt[:, kb * 128:(kb + 1) * 128], qb[:, 0, :], True, True, [bkt, bqb], [bst0])
                    MM(st[:, 512:1024], kt[:, kb * 128:(kb + 1) * 128], qb[:, 1, :], True, True, [bkt, bqb], [bst1], sig=True)
                else:
                    MM(st[:, 0:512], kt[0:64, kb * 128:(kb + 1) * 128], qb[0:64, 0, :], True, True, [bkt, bqb], [bst0])
                    MM(st[:, 512:1024], kt[64:128, kb * 128:(kb + 1) * 128], qb[64:128, 1, :], True, True, [bkt, bqb], [bst1], sig=True)

            li_of = lambda i: li + items[i][0]

            def AVZ(i):
                t_, kb_ = items[i]
                lt_ = li_of(i)
                pt_, bpt_ = PT[i % NPT], bPT[i % NPT]
                vb, bvb = VB[lt_ % NKB], bVB[lt_ % NKB]
                for c in range(2):
                    MM(OACC[c][0], vb[:, kb_, 0:128], pt_[:, c, :], i == 0, i == nit - 1, [bpt_, bvb], [OACC[c][1]])
                for c in range(2):
                    MM(ZACC[c][0], ones_b[:], pt_[:, c, :], i == 0, i == nit - 1, [bpt_] + CR, [ZACC[c][1]], sig=(c == 1))

            QK(0, li_of(0))
            for i, (t, kb) in enumerate(items):
                lt = li_of(i)
                if kb == 0 and lt + 2 < len(loads):
                    issue_load(lt + 2)
                if i + 1 < nit:
                    QK(i + 1, li_of(i + 1))
                st, bst0, bst1 = (PA, bPA0, bPA1) if i % 2 == 0 else (PB, bPB0, bPB1)
                pt, bpt = PT[i % NPT], bPT[i % NPT]
                kbg = 4 * t + kb
                st3 = st[:].rearrange("p (c q) -> p c q", c=2)
                for r in range(nsub):
                    col = ABOFF[(h, s, r)] + kbg
                    ACT(pt[:, :, r * W_:(r + 1) * W_], st3[:, :, r * W_:(r + 1) * W_], AF.Exp, [bst0, bst1, ATTC], [bpt],
                        bias=ABI[:, col:col + 1], scale=0.125, sig=(r == nsub - 1))
                if t >= 4 * s:
                    u = t - 4 * s
                    V("tensor_tensor", [bMK], [bpt], out=pt[:], in0=pt[:],
                      in1=MK[:, u * 4 + kb, :].unsqueeze(1).to_broadcast([128, 2, 512]), op=ALU.mult, sig=True,
                      eng=("pool" if (i % 2 == 1 and os.environ.get("KDBG_MASKENG", "dve") == "mix") else "dve"))
                if i >= 1:
                    AVZ(i - 1)
            AVZ(nit - 1)
            li += KMAX[s]
            for c in range(2):
                ACT(ZC[:, c, :], ZACC[c][0], AF.Copy, [ZACC[c][1]], [bZC], sig=True)
            for c in range(2):
                ACT(OC[:, c, :], OACC[c][0], AF.Copy, [OACC[c][1]], [bOC], sig=True)
            for c in range(2):
                V("reciprocal", [bZC], [bRZ], out=RZ[:, c, :], in_=ZC[:, c, :])
            for c in range(2):
                V("tensor_tensor", [bOC, bRZ], [bFT], out=FT[:, c, :], in0=OC[:, c, :], in1=RZ[:, c, :], op=ALU.mult)
            V("scalar_tensor_tensor", [bFT] + CR, [bYBR[s]], out=YBR[:, h, s * 512:(s + 1) * 512], in0=FT[:, 1, :],
              scalar=neglam[:, 0:1], in1=FT[:, 0, :], op0=ALU.mult, op1=ALU.add)
    S.barrier()
    if upto == "b":
        return nc

    load_w(0, w_up_b)
    YBT = sb("ybT", [128, 8, 512], BF16, R_A + 32768)
    bYBT = Buf("ybT")
    load_act(0, hTs[0], bhTs[0])
    load_act(2, ATs[0], bATs[0])
    MOUT = [sb("mout%d" % i, [128, 8, 512], BF16, R_A + 40960 + i * 8192) for i in range(1)]
    bMOUT = [Buf("mout0")]
    SQB = [sb("sqb%d" % i, [128, 512], BF16, R_A + 57344 + i * 1024) for i in range(2)]
    bSQB = [Buf("sqb%d" % i) for i in range(2)]
    RSD = [sb("rsd%d" % i, [128, 512], F32, R_A + 59392 + i * 2048) for i in range(2)]
    bRSD = [Buf("rsd%d" % i) for i in range(2)]
    SG2 = sb("sgf2", [128, 512], F32, R_A + 49152)
    TMP3 = sb("tmpf3", [128, 512], F32, R_A + 53248)
    bSG2 = Buf("sgf2")
    bTMP3 = Buf("tmpf3")
    YBT2 = [YBT, sb("ybT1", [128, 8, 512], BF16, R_S)]
    bYBT2 = [bYBT, Buf("ybT1")]

    def c3_norm(s, heads=range(8)):
        ybt, bybt = YBT2[s % 2], bYBT2[s % 2]
        for c in heads:
            raw = YBR[:, c, s * 512:(s + 1) * 512]
            pss, bpss = (PC[:, 0:512], bPC0) if c % 2 == 0 else (PC[:, 512:1024], bPC1)
            ACT(SQB[c % 2][:], raw, AF.Square, [bYBR[s]], [bSQB[c % 2]])
            MM(pss, ones_b[:], SQB[c % 2][:], True, True, [bSQB[c % 2]] + CR, [bpss], sig=True)
            ACT(RSD[c % 2][:], pss, AF.Ln, [bpss] + CR, [bRSD[c % 2]], bias=epsT[:, 0:1], scale=1.0 / 128)
            ACT(RSD[c % 2][:], RSD[c % 2][:], AF.Exp, [bRSD[c % 2]], [bRSD[c % 2]], scale=-0.5)
            V("scalar_tensor_tensor", [bYBR[s], bRSD[c % 2]] + CR, [bybt], out=ybt[:, c, :], in0=raw, scalar=gsub8[:, 0:1],
              in1=RSD[c % 2][:], op0=ALU.mult, op1=ALU.mult)

    def c3_mm(s, interleave_next=False):
        ybt, bybt = YBT2[s % 2], bYBT2[s % 2]
        hT, bhT = AIN[s % 2], bAIN[s % 2]
        at, bat = AIN[2 + s % 2], bAIN[2 + s % 2]
        mo, bmo = MOUT[0], bMOUT[0]
        for m in range(8):
            psu, bpsu = (PA[:, 0:512], bPA0) if m % 2 == 0 else (PA[:, 512:1024], bPA1)
            psg, bpsg = (PB[:, 0:512], bPB0) if m % 2 == 0 else (PB[:, 512:1024], bPB1)
            for c in range(8):
                MM(psg, WS4[:, c, m * 128:(m + 1) * 128], hT[:, c, :], c == 0, c == 7, [bhT, bWS4], [bpsg], sig=(c == 7))
            for c in range(8):
                MM(psu, WS[0][:, c, m * 128:(m + 1) * 128], ybt[:, c, :], c == 0, c == 7, [bybt, bWS[0]], [bpsu], sig=(c == 7))
            ACT(SG2[:], psg, AF.Sigmoid, [bpsg] + CR, [bSG2], bias=bgate[:, 8 + m:9 + m])
            V("tensor_tensor", [bSG2, bpsu], [bTMP3], out=TMP3[:], in0=SG2[:], in1=psu, op=ALU.mult)
            V("tensor_tensor", [bTMP3, bat], [bmo], out=mo[:, m, :], in0=TMP3[:], in1=at[:, m, :], op=ALU.add)
            if interleave_next:
                c3_norm(s + 1, heads=[m])
        S.dma("sp", mTs[s], mo[:].rearrange("p c t -> p (c t)"), R=[bmo], W=[bmTs[s]], join=True)

    c3_norm(0)
    for s in range(4):
        if s + 1 < 4:
            load_act((s + 1) % 2, hTs[s + 1], bhTs[s + 1])
            load_act(2 + (s + 1) % 2, ATs[s + 1], bATs[s + 1])
        c3_mm(s, interleave_next=(s + 1 < 4))
    S.barrier()
    if upto == "c3":
        return nc
    X1 = sb("x1", [128, 16, 1024], F32, R_A)
    bX1 = [Buf("x1_%d" % i) for i in range(4)]
    H2T = sb("h2T", [128, 8, 2048], BF16, R_B)
    bH2T = [Buf("h2T_%d" % i) for i in range(4)]
    MIN = [sb("min%d" % i, [128, 8, 512], BF16, R_S + i * 8192) for i in range(2)]
    bMIN = [Buf("min%d" % i) for i in range(2)]
    XS2 = sb("xs2", [128, 4, 1024], F32, R_W)
    bXS2 = Buf("xs2")
    H2F = sb("h2f", [128, 8, 512], F32, R_W + 16384)
    bH2F = Buf("h2f")
    ro = [R_S + 16384]

    def ralloc(name, cols, dt=F32):
        nb = (cols * (4 if dt == F32 else 2) + 31) // 32 * 32
        t = sb(name, [128, cols], dt, ro[0])
        ro[0] += nb
        assert ro[0] <= R_S + 24576, name
        return t

    r_sq = ralloc("r_sq", 1024)
    r_st = ralloc("r_st", 16)
    lg = ralloc("lg", 36)
    r_m = ralloc("r_m", 8)
    r_e = ralloc("r_e", 4)
    r_oh = ralloc("r_oh", 4)
    r_pen = ralloc("r_pen", 4)
    r_em = ralloc("r_em", 32)
    r_oh1 = ralloc("r_oh1", 32)
    r_oh2 = ralloc("r_oh2", 32)
    r_c = ralloc("r_c", 32)
    r_w = ralloc("r_w", 8)
    r_cc = ralloc("r_cc", 64, BF16)
    r_lo = ralloc("r_lo", 32)
    r_c32 = ralloc("r_c32", 64)
    bRT = Buf("rt")
    bCT = Buf("cT")
    S.dma("sp", MIN[0][:].rearrange("p c t -> p (c t)"), mTs[0], R=[bmTs[0]], W=[bMIN[0]])

    def c4_proj(s):
        S.dma("sp", X1[:, s * 4:(s + 1) * 4, :], xo[s * 512:(s + 1) * 512, :].rearrange("(b p) d -> p b d", p=128), W=[bX1[s]])
        if s + 1 < 4:
            S.dma("sp", MIN[(s + 1) % 2][:].rearrange("p c t -> p (c t)"), mTs[s + 1], R=[bmTs[s + 1]], W=[bMIN[(s + 1) % 2]])
        mi, bmi = MIN[s % 2], bMIN[s % 2]
        for tb in range(4):
            for half in range(2):
                ps, bps = (PA[:, 0:512], bPA0) if half == 0 else (PA[:, 512:1024], bPA1)
                for c in range(8):
                    MM(ps, mi[:, c, tb * 128:(tb + 1) * 128], WS[2][:, c, half * 512:(half + 1) * 512], c == 0, c == 7,
                       [bmi, bWS[2]], [bps], sig=(c == 7))
                xv = X1[:, s * 4 + tb, half * 512:(half + 1) * 512]
                V("tensor_tensor", [bps], [bX1[s]], out=xv, in0=xv, in1=ps, op=ALU.add)

    def c4_norm(s):
        for tb in range(4):
            xrow = X1[:, s * 4 + tb, :]
            ACT(r_sq[:], xrow, AF.Square, [bX1[s]], [bRT])
            V("tensor_reduce", [bRT], [bRT], out=r_st[:, 0:1], in_=r_sq[:], axis=AX.X, op=ALU.add)
            ACT(r_st[:, 1:2], r_st[:, 0:1], AF.Sqrt, [bRT] + CR, [bRT], bias=epsT[:, 0:1], scale=1.0 / 1024)
            V("reciprocal", [bRT], [bRT], out=r_st[:, 2:3], in_=r_st[:, 1:2])
            V("tensor_scalar", [bX1[s], bRT], [bXS2], out=XS2[:, tb, :], in0=xrow, scalar1=r_st[:, 2:3], scalar2=None, op0=ALU.mult)

    def c4_tx(s):
        for c in range(8):
            pt, bpt = (PB[:, 0:512], bPB0) if c % 2 == 0 else (PB[:, 512:1024], bPB1)
            for tb in range(4):
                TR(pt[:, tb * 128:(tb + 1) * 128], XS2[:, tb, c * 128:(c + 1) * 128], ident_f[:], [bXS2] + CR, [bpt], sig=(tb == 3))
            V("tensor_scalar", [bpt] + CR, [bH2T[s]], out=H2T[:, c, s * 512:(s + 1) * 512], in0=pt, scalar1=g2c[:, c:c + 1],
              scalar2=None, op0=ALU.mult)
            V("tensor_scalar", [bpt] + CR, [bH2F], out=H2F[:, c, :], in0=pt, scalar1=g2c[:, c:c + 1], scalar2=None, op0=ALU.mult)

    def c4_route(s):
        for tb in range(4):
            for c in range(8):
                MM(PC[:, tb * 36:(tb + 1) * 36], H2F[:, c, tb * 128:(tb + 1) * 128], wr_t[:, c, :], c == 0, c == 7, [bH2F] + CR, [bPC0],
                   sig=(c == 7 and tb == 3))
        B3 = lambda ap, n: ap.unsqueeze(2).to_broadcast([128, 4, n])
        V("tensor_tensor", [bPC0] + CR, [bRT2], out=q_lg[:], in0=PC[:, 0:144].rearrange("p (b e) -> p b e", e=36),
          in1=br_t[:].unsqueeze(1).to_broadcast([128, 4, 36]), op=ALU.add)
        gl = q_lg[:, :, 0:4]
        V("tensor_reduce", [bRT2], [bRT2], out=q_s[:, 0, :], in_=gl, axis=AX.X, op=ALU.max)
        V("tensor_tensor", [bRT2], [bRT2], out=q_ge[:], in0=gl, in1=B3(q_s[:, 0, :], 4), op=ALU.subtract)
        ACT(q_ge[:], q_ge[:], AF.Exp, [bRT2], [bRT2])
        V("tensor_reduce", [bRT2], [bRT2], out=q_s[:, 1, :], in_=q_ge[:], axis=AX.X, op=ALU.add)
        V("reciprocal", [bRT2], [bRT2], out=q_s[:, 2, :], in_=q_s[:, 1, :])
        V("tensor_tensor", [bRT2], [bRT2], out=q_oh[:], in0=gl, in1=B3(q_s[:, 0, :], 4), op=ALU.is_equal)
        V("tensor_scalar", [bRT2], [bRT2], out=q_oh[:], in0=q_oh[:], scalar1=-1.0, scalar2=1e30, op0=ALU.add, op1=ALU.mult)
        V("tensor_tensor", [bRT2], [bRT2], out=q_em[:].rearrange("p b (g e) -> p b g e", e=8),
          in0=q_lg[:, :, 4:36].rearrange("p b (g e) -> p b g e", e=8),
          in1=q_oh[:].unsqueeze(3).to_broadcast([128, 4, 4, 8]), op=ALU.add)
        V("tensor_reduce", [bRT2], [bRT2], out=q_s[:, 3, :], in_=q_em[:], axis=AX.X, op=ALU.max)
        V("tensor_tensor", [bRT2], [bRT2], out=q_o1[:], in0=q_em[:], in1=B3(q_s[:, 3, :], 32), op=ALU.is_equal)
        V("scalar_tensor_tensor", [bRT2], [bRT2], out=q_em[:], in0=q_o1[:], scalar=-1e30, in1=q_em[:], op0=ALU.mult, op1=ALU.add)
        V("tensor_reduce", [bRT2], [bRT2], out=q_s[:, 4, :], in_=q_em[:], axis=AX.X, op=ALU.max)
        V("tensor_tensor", [bRT2], [bRT2], out=q_o2[:], in0=q_em[:], in1=B3(q_s[:, 4, :], 32), op=ALU.is_equal)
        V("tensor_tensor", [bRT2], [bRT2], out=q_s[:, 5, :], in0=q_s[:, 3, :], in1=q_s[:, 4, :], op=ALU.subtract)
        ACT(q_s[:, 6, :], q_s[:, 5, :], AF.Exp, [bRT2], [bRT2], scale=-1.0)
        V("tensor_scalar", [bRT2], [bRT2], out=q_s[:, 6, :], in0=q_s[:, 6, :], scalar1=1.0, scalar2=None, op0=ALU.add)
        V("reciprocal", [bRT2], [bRT2], out=q_s[:, 7, :], in_=q_s[:, 6, :])
        V("tensor_tensor", [bRT2], [bRT2], out=q_s[:, 8, :], in0=q_s[:, 7, :], in1=q_s[:, 2, :], op=ALU.mult)
        V("tensor_tensor", [bRT2], [bRT2], out=q_s[:, 9, :], in0=q_s[:, 2, :], in1=q_s[:, 8, :], op=ALU.subtract)
        V("tensor_tensor", [bRT2], [bRT2], out=q_o1[:], in0=q_o1[:], in1=B3(q_s[:, 8, :], 32), op=ALU.mult)
        V("tensor_tensor", [bRT2], [bRT2], out=q_o2[:], in0=q_o2[:], in1=B3(q_s[:, 9, :], 32), op=ALU.mult)
        V("tensor_tensor", [bRT2], [bRT2], out=q_o1[:], in0=q_o1[:], in1=q_o2[:], op=ALU.add)
        V("tensor_copy", [bRT2], [bRT2], out=q_cb[:], in_=q_o1[:])
        V("tensor_copy", [bRT2], [bRT2], out=q_c32[:, :, 0:32], in_=q_cb[:])
        V("tensor_tensor", [bRT2], [bRT2], out=q_c32[:, :, 32:64], in0=q_o1[:], in1=q_c32[:, :, 0:32], op=ALU.subtract)
        for tb in range(4):
            TR(PD[0:64, 512 + tb * 128:512 + (tb + 1) * 128], q_c32[:, tb, :], ident_f[:], [bRT2] + CR, [bPD1], sig=(tb == 3))
        V("tensor_copy", [bPD1], [bCT], out=cT[:, s * 512:(s + 1) * 512], in_=PD[0:64, 512:1024])

    q_lg = calloc("q_lg", [128, 4, 36], F32)
    q_s = calloc("q_s", [128, 10, 4], F32)
    q_ge = calloc("q_ge", [128, 4, 4], F32)
    q_oh = calloc("q_oh", [128, 4, 4], F32)
    q_em = calloc("q_em", [128, 4, 32], F32)
    q_o1 = calloc("q_o1", [128, 4, 32], F32)
    q_o2 = calloc("q_o2", [128, 4, 32], F32)
    q_cb = calloc("q_cb", [128, 4, 32], BF16)
    q_c32 = calloc("q_c32", [128, 4, 64], F32)
    bRT2 = Buf("rt2")
    c4_proj(0)
    for s in range(4):
        c4_norm(s)
        if s + 1 < 4:
            c4_proj(s + 1)
        c4_tx(s)
        c4_route(s)
    S.barrier()
    if upto == "c4":
        return nc

    EWT = [sb("ew_%d" % i, [128, 6144], BF16, R_W + i * 12288) for i in range(4)]
    EW13 = [t[:, 0:4096].rearrange("p (c f) -> p c f", f=512) for t in EWT]
    EW2 = [t[:, 4096:6144].rearrange("p (f d) -> p f d", d=1024) for t in EWT]
    bEW = [Buf("ew%d" % i) for i in range(4)]
    HID = [sb("hid%d" % i, [128, 2, 512], BF16, R_S + i * 2048) for i in range(4)]
    bHID = [Buf("hid%d" % i) for i in range(4)]
    SA = [sb("sa%d" % i, [128, 512], F32, R_S + 8192 + i * 2048) for i in range(2)]
    bSA = [Buf("sa%d" % i) for i in range(2)]
    TT = [sb("tt%d" % i, [128, 512], F32, R_S + 12288 + i * 2048) for i in range(2)]
    bTT = [Buf("tt%d" % i) for i in range(2)]

    def load_expert(e):
        i = e % 4
        S.dma("pool", EWT[i][:], ewp[e], W=[bEW[i]])

    load_expert(0)
    load_expert(1)
    out_toks = []

    def moe_hid(gp, sbi):
        hcols = slice(sbi * 512, (sbi + 1) * 512)
        for k in range(2):
            e = 2 * gp + k
            wi = e % 4
            hid, bhid = HID[(sbi % 2) * 2 + k], bHID[(sbi % 2) * 2 + k]
            aps = [(PA[:, 0:512], bPA0), (PA[:, 512:1024], bPA1)]
            bps_ = [(PB[:, 0:512], bPB0), (PB[:, 512:1024], bPB1)]
            for fc in range(2):
                for c in range(8):
                    MM(aps[fc][0], EW13[wi][:, c, fc * 128:(fc + 1) * 128], H2T[:, c, hcols], c == 0, c == 7,
                       [bEW[wi], bH2T[sbi]], [aps[fc][1]], sig=(c == 7))
            for fc in range(2):
                for c in range(8):
                    MM(bps_[fc][0], EW13[wi][:, c, 256 + fc * 128:256 + (fc + 1) * 128], H2T[:, c, hcols], c == 0, c == 7,
                       [bEW[wi], bH2T[sbi]], [bps_[fc][1]], sig=(c == 7))
            MM(PD[:, 0:512], sel_t[:, e, :], cT[:, hcols], True, True, [bCT] + CR, [bPD], sig=True)
            for fc in range(2):
                ACT(SA[fc][:], aps[fc][0], AF.Silu, [aps[fc][1]], [bSA[fc]])
                V("tensor_tensor", [bSA[fc], bps_[fc][1]], [bTT[fc]], out=TT[fc][:], in0=SA[fc][:], in1=bps_[fc][0], op=ALU.mult)
                V("tensor_tensor", [bTT[fc], bPD], [bhid], out=hid[:, fc, :], in0=TT[fc][:], in1=PD[:, 0:512], op=ALU.mult)

    def moe_y(gp, sbi):
        for tb in range(4):
            for half in range(2):
                ps, bps = (PC[:, 0:512], bPC0) if half == 0 else (PC[:, 512:1024], bPC1)
                n = 0
                for k in range(2):
                    e = 2 * gp + k
                    wi = e % 4
                    hid, bhid = HID[(sbi % 2) * 2 + k], bHID[(sbi % 2) * 2 + k]
                    for fc in range(2):
                        MM(ps, hid[:, fc, tb * 128:(tb + 1) * 128], EW2[wi][:, fc, half * 512:(half + 1) * 512], n == 0, n == 3,
                           [bhid, bEW[wi]], [bps], sig=(n == 3))
                        n += 1
                xv = X1[:, sbi * 4 + tb, half * 512:(half + 1) * 512]
                V("tensor_tensor", [bps], [bX1[sbi]], out=xv, in0=xv, in1=ps, op=ALU.add)
        if gp == NE // 2 - 1:
            out_toks.append(S.dma("sp", out[sbi * 512:(sbi + 1) * 512, :].rearrange("(b p) d -> p b d", p=128),
                                  X1[:, sbi * 4:(sbi + 1) * 4, :], R=[bX1[sbi]]))

    steps = [(gp, sbi) for gp in range(NE // 2) for sbi in range(4)]
    for i, (gp, sbi) in enumerate(steps):
        moe_hid(gp, sbi)
        if i >= 1:
            moe_y(*steps[i - 1])
        if sbi == 0 and gp + 1 < NE // 2:
            load_expert(2 * gp + 2)
            load_expert(2 * gp + 3)
    moe_y(*steps[-1])
    S.finish(out_toks)
    return nc


_NC_CACHE = {}


def _host_consts(inp):
    f = np.float32
    L = 0
    c = {}
    c["c_ident"] = np.eye(128, dtype=f)
    c["c_g1"] = np.ascontiguousarray(inp["norm1_g"][L].reshape(8, 128).T)
    c["c_g2"] = np.ascontiguousarray(inp["norm2_g"][L].reshape(8, 128).T)
    gq = np.concatenate([inp["q_norm_g"][L], inp["q_norm_g"][L]])
    gk = np.concatenate([inp["k_norm_g"][L], inp["k_norm_g"][L]])
    c["c_gqk"] = np.ascontiguousarray(np.stack([gq, gk], axis=1).astype(f))
    c["c_gv"] = np.ascontiguousarray(np.broadcast_to(inp["v_norm_g"][L].reshape(1, 1024), (128, 1024)))
    c["c_wsT"] = np.ascontiguousarray(inp["w_s"][L].transpose(2, 0, 1))
    c["c_triu"] = np.triu(np.ones((128, 128), f))
    c["c_bs"] = np.ascontiguousarray(np.broadcast_to(inp["b_s"][L][None], (128, 8, 128)))
    c["c_bgate"] = np.ascontiguousarray(inp["b_gate"][L].reshape(16, 128).T)
    c["c_gsub"] = np.ascontiguousarray(inp["sub_norm_g"][L].reshape(128, 1))
    lam = np.stack([inp["lambda_q1"][L], inp["lambda_k1"][L], inp["lambda_q2"][L], inp["lambda_k2"][L]])
    c["c_lam"] = np.ascontiguousarray(np.broadcast_to(lam[None], (128, 4, 64)))
    br = np.concatenate([inp["b_rg"][L], inp["b_re"][L]])
    c["c_br"] = np.ascontiguousarray(np.broadcast_to(br[None], (128, 36)))
    wr = np.concatenate([inp["w_rg"][L], inp["w_re"][L]], axis=1)
    c["c_wr"] = np.ascontiguousarray(wr.reshape(8, 128, 36).transpose(1, 0, 2))
    sel = np.zeros((64, NE, 128), f)
    for e in range(NE):
        sel[e, e, :] = 1.0
        sel[32 + e, e, :] = 1.0
    c["c_sel"] = sel
    k = np.arange(128)[:, None, None]
    kb = np.arange(4)[None, :, None]
    q = np.arange(512)[None, None, :]
    c["c_tri4"] = ((128 * kb + k) <= q).astype(f)
    return c


def _core_tables(j):
    f = np.float32
    sbs = [j, 7 - j, 8 + j, 15 - j]
    mcoef = np.zeros((32,), f)
    for s in range(4):
        for u in range(4):
            t = 4 * s + u
            a, b = (1.0, 0.0) if t < sbs[s] else ((0.0, 1.0) if t == sbs[s] else (0.0, 0.0))
            mcoef[(s * 4 + u) * 2] = a
            mcoef[(s * 4 + u) * 2 + 1] = b
    ab = np.zeros((128, NAB), f)
    p = np.arange(128, dtype=np.float64)
    for h in range(8):
        W = WH[h]
        for s in range(4):
            for r in range(512 // W):
                ref = 512 * sbs[s] + r * W + W / 2
                base = ABOFF[(h, s, r)]
                for kbg in range(4 * KMAX[s]):
                    v = SLOPES[h] * (128 * kbg + p - ref)
                    ab[:, base + kbg] = np.minimum(v, SLOPES[h] * W / 2)
    return sbs, np.ascontiguousarray(np.broadcast_to(mcoef[None], (128, 32))), ab


def kernel(**inputs):
    inp = {k: np.asarray(v) for k, v in inputs.items()}
    x = inp["x"]
    if "nc" not in _NC_CACHE:
        _NC_CACHE["nc"] = build_nc()
    nc = _NC_CACHE["nc"]
    consts = _host_consts(inp)
    shared = {
        "w_in": np.ascontiguousarray(inp["w_in"][0]), "w_gate": np.ascontiguousarray(inp["w_gate"][0]),
        "w_up_a": np.ascontiguousarray(inp["w_up_a"][0]), "w_up_b": np.ascontiguousarray(inp["w_up_b"][0]),
        "w_out": np.ascontiguousarray(inp["w_out"][0]),
    }
    w13 = np.concatenate([inp["w1"][0].reshape(NE, 8, 128, 256), inp["w3"][0].reshape(NE, 8, 128, 256)], axis=3)
    w13 = w13.transpose(0, 2, 1, 3).reshape(NE, 128, 4096)
    w2p = inp["w2"][0].reshape(NE, 2, 128, 1024).transpose(0, 2, 1, 3).reshape(NE, 128, 2048)
    shared["ewp"] = np.ascontiguousarray(np.concatenate([w13, w2p], axis=2))
    shared.update(consts)
    in_maps = []
    own = []
    for c in range(8):
        b, j = c // 4, c % 4
        sbs, mcoef, ab = _core_tables(j)
        xo = np.concatenate([x[b, sb * 512:(sb + 1) * 512] for sb in sbs], axis=0)
        m = dict(shared)
        m["xf"] = np.ascontiguousarray(x[b])
        m["xo"] = np.ascontiguousarray(xo)
        m["c_mcoef"] = mcoef
        m["c_abias"] = ab
        in_maps.append(m)
        own.append((b, sbs))
    res = run_bass_kernel_spmd(nc, in_maps, core_ids=list(range(8)))
    outp = np.empty((2, SEQ, D), np.float32)
    for c in range(8):
        b, sbs = own[c]
        o = np.asarray(res.results[c]["out"])
        for i, sb in enumerate(sbs):
            outp[b, sb * 512:(sb + 1) * 512] = o[i * 512:(i + 1) * 512]
    return outp
```
